# Optimizing a Trainium2 kernel written in Bass

```python
import math
import jax
import jax.numpy as jnp
from jax import lax
import numpy as np

D_MODEL = 2048
BATCH = 8
SEQ = 2048
DEPTH = 2

CHUNK = 64
Q_BLOCK = 128
EPS = 1e-6

GLA_DV = 128
GLA_DK = GLA_DV // 2
GLA_WIDTH = 3 * D_MODEL // 8
GLA_HEADS = GLA_WIDTH // GLA_DV
GLA_KEY_WIDTH = GLA_HEADS * GLA_DK
GLA_LOWRANK = 16
GLA_TAU = 16.0

LRU_WIDTH = D_MODEL // 4
LRU_BLOCKS = 8
LRU_BLOCK_DIM = LRU_WIDTH // LRU_BLOCKS
CONV_WIDTH = 4
LRU_C = 8.0

DIFF_DH = 64
DIFF_DV = 2 * DIFF_DH
DIFF_WIDTH = D_MODEL - GLA_WIDTH - LRU_WIDTH
DIFF_HEADS = DIFF_WIDTH // DIFF_DV
DIFF_QK_WIDTH = DIFF_HEADS * 2 * DIFF_DH

MIX_WIDTH = GLA_WIDTH + LRU_WIDTH + DIFF_WIDTH

REL_BUCKETS = 32
REL_MAX_DIST = 128

N_GROUPS = 8
EXPERTS_PER_GROUP = 8
N_EXPERTS = N_GROUPS * EXPERTS_PER_GROUP
TOP_K = 2
D_EXPERT = D_MODEL // 4
MOE_BLOCK = 128

IN_SIZES = (GLA_KEY_WIDTH, GLA_KEY_WIDTH, GLA_WIDTH, GLA_WIDTH, GLA_LOWRANK,
            LRU_WIDTH, LRU_WIDTH,
            DIFF_QK_WIDTH, DIFF_QK_WIDTH, DIFF_WIDTH)
IN_WIDTH = sum(IN_SIZES)
IN_SPLITS = tuple(int(s) for s in np.cumsum(IN_SIZES)[:-1])

kernel_name = "hybrid_gla_rglru_diffattn_hmoe"


def rmsnorm(x, g):
    xf = x.astype(jnp.float32)
    y = xf * lax.rsqrt(jnp.mean(xf * xf, axis=-1, keepdims=True) + EPS)
    return (y * g.astype(jnp.float32)).astype(x.dtype)


def head_rms(o):
    return o * lax.rsqrt(jnp.mean(o * o, axis=-1, keepdims=True) + EPS)


def t5_bucket(rel):
    nb = REL_BUCKETS // 2
    ret = (rel > 0).astype(jnp.int32) * nb
    n = jnp.abs(rel)
    max_exact = nb // 2
    nf = jnp.maximum(n, 1).astype(jnp.float32)
    large = max_exact + (jnp.log(nf / max_exact) / math.log(REL_MAX_DIST / max_exact)
                         * (nb - max_exact)).astype(jnp.int32)
    large = jnp.minimum(large, nb - 1)
    return ret + jnp.where(n < max_exact, n, large)


def gla_mixer(q, k, v, og, a_lr, w_a2, b_a, norm_g):
    f32 = jnp.float32
    B, S, _ = q.shape
    N = S // CHUNK
    log_alpha = jax.nn.log_sigmoid(a_lr.astype(f32) @ w_a2.astype(f32) + b_a.astype(f32)) / GLA_TAU

    def heads(t, d):
        return t.astype(f32).reshape(B, N, CHUNK, GLA_HEADS, d).transpose(0, 3, 1, 2, 4)

    qh = heads(q, GLA_DK) * (GLA_DK ** -0.5)
    kh = heads(k, GLA_DK)
    vh = heads(v, GLA_DV)
    G = jnp.cumsum(heads(log_alpha, GLA_DK), axis=3)
    G_last = G[:, :, :, -1:, :]
    eG = jnp.exp(G)
    enG = jnp.exp(-G)
    q_fwd = qh * eG
    a_fwd = jnp.einsum('bhnid,bhnjd->bhnij', q_fwd, kh * enG)
    a_bwd = jnp.einsum('bhnid,bhnjd->bhnij', qh * enG, kh * eG)
    causal = jnp.tril(jnp.ones((CHUNK, CHUNK), dtype=bool))
    attn = jnp.where(causal, a_fwd, a_bwd)
    o_intra = jnp.einsum('bhnij,bhnjv->bhniv', attn, vh)
    kv = jnp.einsum('bhncd,bhncv->bhndv', kh * jnp.exp(G_last - G), vh)
    decay = jnp.exp(G_last[:, :, :, 0, :])

    def step(state, inp):
        kv_n, dec_n = inp
        return dec_n[..., None] * state + kv_n, state

    init = jnp.zeros((B, GLA_HEADS, GLA_DK, GLA_DV), f32)
    _, s_prev = lax.scan(step, init, (jnp.moveaxis(kv, 2, 0), jnp.moveaxis(decay, 2, 0)))
    s_prev = jnp.moveaxis(s_prev, 0, 2)
    o_inter = jnp.einsum('bhncd,bhndv->bhncv', q_fwd, s_prev)
    o = (o_intra + o_inter).transpose(0, 2, 3, 1, 4).reshape(B, S, GLA_HEADS, GLA_DV)
    o = head_rms(o).reshape(B, S, GLA_WIDTH) * norm_g.astype(f32)
    return o * jax.nn.silu(og.astype(f32))


def rglru_mixer(y_br, x_br, conv_w, conv_b, wa, ba, wx, bx, lam):
    f32 = jnp.float32
    B, S, W = x_br.shape
    xp = jnp.pad(x_br.astype(f32), ((0, 0), (CONV_WIDTH - 1, 0), (0, 0)))
    xc = conv_b.astype(f32)
    for i in range(CONV_WIDTH):
        xc = xc + xp[:, i:i + S] * conv_w[i].astype(f32)
    xb = xc.reshape(B, S, LRU_BLOCKS, LRU_BLOCK_DIM)
    r = jax.nn.sigmoid(jnp.einsum('bsnd,nde->bsne', xb, wa.astype(f32)).reshape(B, S, W) + ba.astype(f32))
    ig = jax.nn.sigmoid(jnp.einsum('bsnd,nde->bsne', xb, wx.astype(f32)).reshape(B, S, W) + bx.astype(f32))
    log_a = -LRU_C * r * jax.nn.softplus(-lam.astype(f32))
    a = jnp.exp(log_a)
    b = jnp.sqrt(-jnp.expm1(2.0 * log_a)) * (ig * xc)

    def combine(lhs, rhs):
        a1, b1 = lhs
        a2, b2 = rhs
        return a1 * a2, a2 * b1 + b2

    _, h = lax.associative_scan(combine, (a, b), axis=1)
    return h * jax.nn.gelu(y_br.astype(f32))


def diff_attention(q, k, v, lq1, lk1, lq2, lk2, subln_g, rel_table, layer_idx):
    f32 = jnp.float32
    B, S, _ = q.shape
    qh = q.astype(f32).reshape(B, S, DIFF_HEADS, 2, DIFF_DH).transpose(0, 2, 3, 1, 4) * (DIFF_DH ** -0.5)
    kh = k.astype(f32).reshape(B, S, DIFF_HEADS, 2, DIFF_DH).transpose(0, 2, 3, 1, 4)
    vh = v.astype(f32).reshape(B, S, DIFF_HEADS, DIFF_DV).transpose(0, 2, 1, 3)
    lam_init = 0.8 - 0.6 * math.exp(-0.3 * layer_idx)
    lam = (jnp.exp(jnp.sum(lq1.astype(f32) * lk1.astype(f32)))
           - jnp.exp(jnp.sum(lq2.astype(f32) * lk2.astype(f32))) + lam_init)
    table = rel_table.astype(f32)
    pos = jnp.arange(S, dtype=jnp.int32)
    outs = []
    for blk in range(S // Q_BLOCK):
        q0 = blk * Q_BLOCK
        kv_len = q0 + Q_BLOCK
        qp = pos[q0:kv_len]
        kp = pos[:kv_len]
        bias = table[t5_bucket(kp[None, :] - qp[:, None])].transpose(2, 0, 1)[:, None]
        mask = (kp[None, :] // CHUNK) <= (qp[:, None] // CHUNK)
        logits = jnp.einsum('bhmqd,bhmkd->bhmqk', qh[:, :, :, q0:kv_len], kh[:, :, :, :kv_len]) + bias
        p = jax.nn.softmax(jnp.where(mask, logits, -1e30), axis=-1)
        attn = p[:, :, 0] - lam * p[:, :, 1]
        outs.append(jnp.einsum('bhqk,bhkv->bhqv', attn, vh[:, :, :kv_len]))
    o = jnp.concatenate(outs, axis=2)
    o = head_rms(o) * subln_g.astype(f32) * (1.0 - lam_init)
    return o.transpose(0, 2, 1, 3).reshape(B, S, DIFF_WIDTH)


def hier_moe(u, wg, bg, we, be, w1, w3, w2):
    f32 = jnp.float32
    B, S, D = u.shape
    T = B * S
    xt = u.reshape(T, D)
    tok_ids = jnp.arange(T, dtype=jnp.int32)
    g_logits = (xt @ wg + bg).astype(f32)
    g_prob = jax.nn.softmax(g_logits, axis=-1)
    g_idx = jnp.argmax(g_logits, axis=-1).astype(jnp.int32)
    g_w = g_prob[tok_ids, g_idx]
    e_all = (xt @ we + be).astype(f32).reshape(T, N_GROUPS, EXPERTS_PER_GROUP)
    e_logits = e_all[tok_ids, g_idx]
    top_v, top_i = lax.top_k(e_logits, TOP_K)
    e_w = jax.nn.softmax(top_v, axis=-1)
    expert = (g_idx[:, None] * EXPERTS_PER_GROUP + top_i).reshape(-1)
    weight = (g_w[:, None] * e_w).reshape(-1)
    tok = jnp.repeat(tok_ids, TOP_K)
    A = T * TOP_K
    order = jnp.argsort(expert)
    e_sorted = expert[order]
    counts = jnp.bincount(expert, length=N_EXPERTS)
    starts = jnp.cumsum(counts) - counts
    padded = (counts + MOE_BLOCK - 1) // MOE_BLOCK * MOE_BLOCK
    pends = jnp.cumsum(padded)
    pstarts = pends - padded
    dest = pstarts[e_sorted] + (jnp.arange(A, dtype=jnp.int32) - starts[e_sorted])
    P = A + N_EXPERTS * MOE_BLOCK
    n_blocks = P // MOE_BLOCK
    slot_tok = jnp.zeros((P,), jnp.int32).at[dest].set(tok[order])
    slot_w = jnp.zeros((P,), f32).at[dest].set(weight[order])
    block_start = jnp.arange(n_blocks, dtype=pends.dtype) * MOE_BLOCK
    block_expert = jnp.minimum(jnp.searchsorted(pends, block_start, side='right'), N_EXPERTS - 1)
    xs = xt[slot_tok].reshape(n_blocks, MOE_BLOCK, D)

    def run_block(args):
        xb, e = args
        hid = jax.nn.silu(xb @ w1[e]) * (xb @ w3[e])
        return hid @ w2[e]

    y = lax.map(run_block, (xs, block_expert)).reshape(P, D)
    out = jnp.zeros((T, D), f32).at[slot_tok].add(y.astype(f32) * slot_w[:, None])
    return out.reshape(B, S, D).astype(u.dtype)


def setup_inputs(seed: int = 0) -> dict:
    key = jax.random.key(seed)
    ks = jax.random.split(key, 32)
    L, D = DEPTH, D_MODEL

    def nrm(k, shape, scale):
        return jax.random.normal(k, shape, jnp.float32) * scale

    u = jax.random.uniform(ks[15], (L, LRU_WIDTH), jnp.float32, minval=0.9, maxval=0.999)
    a0 = u ** (1.0 / LRU_C)
    return {
        "x": nrm(ks[0], (BATCH, SEQ, D), 1.0),
        "c": nrm(ks[1], (BATCH, D), 1.0),
        "ada_w": nrm(ks[2], (L, D, 6 * D), 0.5 * D ** -0.5),
        "ada_b": nrm(ks[3], (L, 6 * D), 0.02),
        "norm1_g": 1.0 + nrm(ks[4], (L, D), 0.02),
        "w_in": nrm(ks[5], (L, D, IN_WIDTH), D ** -0.5),
        "gla_w_a2": nrm(ks[6], (L, GLA_LOWRANK, GLA_KEY_WIDTH), GLA_LOWRANK ** -0.5),
        "gla_b_a": nrm(ks[7], (L, GLA_KEY_WIDTH), 0.02),
        "gla_norm_g": 1.0 + nrm(ks[8], (L, GLA_WIDTH), 0.02),
        "lru_conv_w": nrm(ks[9], (L, CONV_WIDTH, LRU_WIDTH), CONV_WIDTH ** -0.5),
        "lru_conv_b": nrm(ks[10], (L, LRU_WIDTH), 0.02),
        "lru_wa": nrm(ks[11], (L, LRU_BLOCKS, LRU_BLOCK_DIM, LRU_BLOCK_DIM), LRU_BLOCK_DIM ** -0.5),
        "lru_ba": nrm(ks[12], (L, LRU_WIDTH), 0.02),
        "lru_wx": nrm(ks[13], (L, LRU_BLOCKS, LRU_BLOCK_DIM, LRU_BLOCK_DIM), LRU_BLOCK_DIM ** -0.5),
        "lru_bx": nrm(ks[14], (L, LRU_WIDTH), 0.02),
        "lru_lambda": jnp.log(a0) - jnp.log1p(-a0),
        "diff_lq1": nrm(ks[16], (L, DIFF_DH), 0.1),
        "diff_lk1": nrm(ks[17], (L, DIFF_DH), 0.1),
        "diff_lq2": nrm(ks[18], (L, DIFF_DH), 0.1),
        "diff_lk2": nrm(ks[19], (L, DIFF_DH), 0.1),
        "diff_subln_g": 1.0 + nrm(ks[20], (L, DIFF_DV), 0.02),
        "rel_bias": nrm(ks[21], (REL_BUCKETS, DIFF_HEADS), 0.5),
        "w_out": nrm(ks[22], (L, MIX_WIDTH, D), MIX_WIDTH ** -0.5),
        "norm2_g": 1.0 + nrm(ks[23], (L, D), 0.02),
        "router_g_w": nrm(ks[24], (L, D, N_GROUPS), D ** -0.5),
        "router_g_b": nrm(ks[25], (L, N_GROUPS), 0.01),
        "router_e_w": nrm(ks[26], (L, D, N_EXPERTS), D ** -0.5),
        "router_e_b": nrm(ks[27], (L, N_EXPERTS), 0.01),
        "moe_w1": nrm(ks[28], (L, N_EXPERTS, D, D_EXPERT), D ** -0.5),
        "moe_w3": nrm(ks[29], (L, N_EXPERTS, D, D_EXPERT), D ** -0.5),
        "moe_w2": nrm(ks[30], (L, N_EXPERTS, D_EXPERT, D), D_EXPERT ** -0.5),
        "final_g": 1.0 + nrm(ks[31], (D,), 0.02),
    }


def reference(x, c, ada_w, ada_b, norm1_g, w_in, gla_w_a2, gla_b_a, gla_norm_g,
              lru_conv_w, lru_conv_b, lru_wa, lru_ba, lru_wx, lru_bx, lru_lambda,
              diff_lq1, diff_lk1, diff_lq2, diff_lk2, diff_subln_g, rel_bias, w_out,
              norm2_g, router_g_w, router_g_b, router_e_w, router_e_b,
              moe_w1, moe_w3, moe_w2, final_g):
    h = x
    for l in range(DEPTH):
        mod = jax.nn.silu(c) @ ada_w[l] + ada_b[l]
        sh1, sc1, g1, sh2, sc2, g2 = jnp.split(mod[:, None, :], 6, axis=-1)
        u = rmsnorm(h, norm1_g[l]) * (1.0 + sc1) + sh1
        proj = u @ w_in[l]
        gq, gk, gv, gog, ga, ly, lx, dq, dk, dv = jnp.split(proj, IN_SPLITS, axis=-1)
        o_gla = gla_mixer(gq, gk, gv, gog, ga, gla_w_a2[l], gla_b_a[l], gla_norm_g[l])
        o_lru = rglru_mixer(ly, lx, lru_conv_w[l], lru_conv_b[l], lru_wa[l], lru_ba[l],
                            lru_wx[l], lru_bx[l], lru_lambda[l])
        o_diff = diff_attention(dq, dk, dv, diff_lq1[l], diff_lk1[l], diff_lq2[l], diff_lk2[l],
                                diff_subln_g[l], rel_bias, l)
        mixed = jnp.concatenate([o_gla, o_lru, o_diff], axis=-1).astype(u.dtype)
        h = h + g1 * (mixed @ w_out[l])
        u = rmsnorm(h, norm2_g[l]) * (1.0 + sc2) + sh2
        h = h + g2 * hier_moe(u, router_g_w[l], router_g_b[l], router_e_w[l], router_e_b[l],
                              moe_w1[l], moe_w3[l], moe_w2[l])
    return rmsnorm(h, final_g)
```

```python
import math
import numpy as np
from contextlib import ExitStack
import ml_dtypes
import concourse.bass as bass
import concourse.mybir as mybir
from concourse.bass_utils import run_bass_kernel_spmd

F32 = mybir.dt.float32
BF16 = mybir.dt.bfloat16
U32 = mybir.dt.uint32
U8 = mybir.dt.uint8
AF = mybir.ActivationFunctionType
ALU = mybir.AluOpType
AX = mybir.AxisListType

D = 2048
S = 2048
NT = 16
KC = 16
L = 2
EPS = 1e-6
INW = 5648
OFF_GQ, OFF_GK, OFF_GV, OFF_GOG, OFF_GA, OFF_LY, OFF_LX, OFF_DQ, OFF_DK, OFF_DV = (
    0, 384, 768, 1536, 2304, 2320, 2832, 3344, 4112, 4880)
NEXP = 64
DEXP = 512


class Tok:
    __slots__ = ("name", "w", "r", "ex")

    def __init__(self, name="", ex=False):
        self.name = name
        self.w = None
        self.r = {}
        self.ex = ex


class _Eng:
    def __init__(self, name, key):
        self.name = name
        self.key = key
        self.n = 0
        self.stream = []
        self.known = {}


class Prog:
    NDS = 32

    def __init__(self, nc, stack):
        self.nc = nc
        self.stack = stack
        self.sems = {}
        self.eng = {}
        for name in ("pe", "act", "dve", "pool", "sp"):
            key = "E_" + name
            self.sems[key] = stack.enter_context(nc.semaphore("s_" + name))
            self.eng[name] = _Eng(name, key)
        self.dkeys = []
        self.dcount = []
        for i in range(self.NDS):
            key = "D_%d" % i
            self.sems[key] = stack.enter_context(nc.semaphore("d_%d" % i))
            self.dkeys.append(key)
            self.dcount.append(0)
        self.drr = 0
        self.ncc = 0
        self._tid = 0

    def tok(self, name="", ex=False):
        self._tid += 1
        return Tok(name or "t%d" % self._tid, ex)

    def _waits(self, e, reads, writes, is_dma):
        need = {}

        def add(ev):
            if ev is None:
                return
            k, v = ev
            if need.get(k, 0) < v:
                need[k] = v

        for t in reads:
            add(t.w)
            if t.ex:
                for k, v in t.r.items():
                    if k != e.key:
                        add((k, v))
        for t in writes:
            if t.w is not None and (is_dma or t.w[0] != e.key):
                add(t.w)
            for k, v in t.r.items():
                if is_dma or k != e.key:
                    add((k, v))
        for k, v in need.items():
            if e.known.get(k, 0) < v:
                e.known[k] = v
                e.stream.append(("wait", k, v))

    def _mark(self, ev, reads, writes):
        k, v = ev
        for t in reads:
            if t.r.get(k, 0) < v:
                t.r[k] = v
        for t in writes:
            t.w = ev
            t.r = {}

    def op(self, en, fn, reads=(), writes=()):
        e = self.eng[en]
        self._waits(e, reads, writes, False)
        e.n += 1
        e.stream.append(("ins", fn, e.key, 1))
        self._mark((e.key, e.n), reads, writes)

    def dma(self, qn, out, in_, reads=(), writes=(), **kw):
        self.custom(qn, (lambda eng: eng.dma_start(out=out, in_=in_, **kw)), reads, writes)

    def custom(self, qn, fn, reads=(), writes=()):
        e = self.eng[qn]
        i = self.drr
        self.drr = (i + 1) % self.NDS
        k = self.dkeys[i]
        if self.dcount[i] > 0:
            v = self.dcount[i] * 16
            if e.known.get(k, 0) < v:
                e.known[k] = v
                e.stream.append(("wait", k, v))
        self._waits(e, reads, writes, True)
        self.dcount[i] += 1
        e.stream.append(("ins", fn, k, 16))
        self._mark((k, self.dcount[i] * 16), reads, writes)

    def collective(self, fn, reads=(), writes=()):
        e = self.eng["pool"]
        key = "C_%d" % self.ncc
        self.ncc += 1
        self.sems[key] = self.stack.enter_context(self.nc.semaphore("c_%d" % self.ncc))
        self._waits(e, reads, writes, True)
        e.stream.append(("ins", fn, key, 1))
        self._mark((key, 1), reads, writes)

    def barrier(self):
        evs = []
        for o in self.eng.values():
            if o.n > 0:
                evs.append((o.key, o.n))
        for i, k in enumerate(self.dkeys):
            if self.dcount[i] > 0:
                evs.append((k, self.dcount[i] * 16))
        for i in range(self.ncc):
            evs.append(("C_%d" % i, 1))
        for e in self.eng.values():
            for k, v in evs:
                if k == e.key:
                    continue
                if e.known.get(k, 0) < v:
                    e.known[k] = v
                    e.stream.append(("wait", k, v))

    def emit(self):
        nc = self.nc
        self.barrier()

        def replay(e, eng):
            for it in e.stream:
                if it[0] == "wait":
                    eng.wait_ge(self.sems[it[1]], it[2])
                else:
                    ins = it[1](eng)
                    ins.then_inc(self.sems[it[2]], it[3])

        with nc.Block() as block:
            @block.tensor
            def _(eng):
                replay(self.eng["pe"], eng)

            @block.scalar
            def _(eng):
                replay(self.eng["act"], eng)

            @block.vector
            def _(eng):
                replay(self.eng["dve"], eng)

            @block.gpsimd
            def _(eng):
                replay(self.eng["pool"], eng)

            @block.sync
            def _(eng):
                replay(self.eng["sp"], eng)


_DTSZ = {F32: 4, BF16: 2, U32: 4, U8: 1}


class Arena:
    def __init__(self, nc, nbytes):
        self.t = nc.alloc_sbuf_tensor("arena", [128, nbytes], U8)
        self.cap = nbytes
        self.off = 0
        self.peak = 0

    def alloc(self, shape, dt):
        sz = _DTSZ[dt]
        n = int(np.prod(shape[1:])) * sz
        n_al = (n + 31) // 32 * 32
        assert self.off + n_al <= self.cap, ("arena overflow", self.off, n_al, self.cap)
        ap = self.t[0:shape[0], self.off:self.off + n].bitcast(dt)
        if len(shape) == 3:
            ap = ap.rearrange("p (a b) -> p a b", a=shape[1])
        elif len(shape) == 4:
            ap = ap.rearrange("p (a b c) -> p a b c", a=shape[1], b=shape[2])
        self.off += n_al
        self.peak = max(self.peak, self.off)
        return ap

    def mark(self):
        return self.off

    def release(self, m):
        self.off = m


GDBG = {"stop": 99, "nhp": 3, "nt": 16}


def build(R, stages, dbg=()):
    nc = bass.Bass("TRN2", target_bir_lowering=False)
    st = ExitStack()
    P = Prog(nc, st)
    AR = Arena(nc, 206 * 1024)
    JC = 96 // R

    def din(name, shape, dt=F32):
        return nc.dram_tensor(name, list(shape), dt, kind="ExternalInput")

    def dscr(name, shape, dt=F32):
        return nc.dram_tensor(name, list(shape), dt)

    x_d = din("x", [S, D])
    c_col_d = din("c_col", [128, KC, 8])
    onehot_d = din("onehot_rep", [128, R * L * JC, 8])
    ada_w_d = din("ada_w_sh", [L, D, 12288 // R])
    ada_b_d = din("ada_b_col", [128, L, 96])
    normg_d = din("norm_g_col", [128, L, 2, KC])
    finalg_d = din("final_g_col", [128, KC])
    ident_d = din("ident_f", [128, 128])
    ones_d = din("ones_f", [128, 128])
    w_in_d = din("w_in_sh", [L, D // R, INW])
    w_out_d = din("w_out_sh", [L, D // R, D])
    if "lru" in stages:
        lru_col_d = din("lru_col", [128, L, 4, 8])
        lru_wbd_d = din("lru_wbd", [L, 4, 2, 128, 128])
    if "gla" in stages:
        gla_wa2_d = din("gla_wa2", [L, 17, 384])
        gla_ng_d = din("gla_ng", [L, 768])
        tri_d = din("gla_tri", [3, 64, 64])
        gmask_d = din("gla_mask", [2, 64, 192])
    if "diff" in stages:
        diff_l_d = din("diff_l", [L, 256])
        subln_d = din("diff_subln", [L, 128])
        relb_d = din("rel_bias", [1, 192])
        t5oh_d = din("t5_oh", [128, 32, 256])
        t5mask_d = din("t5_mask", [128, 256])
    if "moe" in stages:
        router_w_d = din("router_w", [L, D, 72])
        router_b_d = din("router_b", [L, 72])
        moe_w1_d = din("moe_w1_sh", [L, NEXP // R, D, DEXP])
        moe_w3_d = din("moe_w3_sh", [L, NEXP // R, D, DEXP])
        moe_w2_d = din("moe_w2_sh", [L, NEXP // R, DEXP, D])
    out_d = nc.dram_tensor("out", [S, D], F32, kind="ExternalOutput")
    dbg_out = {}

    hT_d = dscr("hT", [D, S])
    mixT_d = dscr("mixT", [D, S], BF16)
    hT_v = hT_d.ap().rearrange("(kc p) t -> p kc t", p=128)
    mixT_v = mixT_d.ap().rearrange("(kc p) t -> p kc t", p=128)

    NFB = 6
    PB = [nc.alloc_psum_tensor("pb%d" % i, [128, 512], F32) for i in range(NFB)]
    PT = [P.tok("pb%d" % i, ex=True) for i in range(NFB)]
    PBT = [nc.alloc_psum_tensor("pbt%d" % i, [128, 1024], BF16) for i in range(2)]
    PTT = [P.tok("pbt0", ex=True), P.tok("pbt1", ex=True)]
    bk = [0]
    bkt = [0]

    nrot = [NFB]

    def bank():
        i = bk[0] % nrot[0]
        bk[0] = (i + 1) % nrot[0]
        return i

    def tbank():
        i = bkt[0]
        bkt[0] = (i + 1) % 2
        return PBT[i], PTT[i]

    def MM(out, lhsT, rhs, start, stop, reads, writes):
        P.op("pe", lambda e: e.matmul(out, lhsT, rhs, start=start, stop=stop), reads, writes)

    def TR(out, in_, ident, reads, writes):
        P.op("pe", lambda e: e.transpose(out, in_, ident), reads, writes)

    def ACT(out, in_, func, reads, writes, **kw):
        P.op("act", lambda e: e.activation(out=out, in_=in_, func=func, **kw), reads, writes)

    def TT(en, out, in0, in1, op, reads, writes):
        P.op(en, lambda e: e.tensor_tensor(out=out, in0=in0, in1=in1, op=op), reads, writes)

    def TS(en, out, in0, s1, s2, op0, op1, reads, writes):
        if op1 is None:
            P.op(en, lambda e: e.tensor_scalar(out=out, in0=in0, scalar1=s1, scalar2=None, op0=op0), reads, writes)
        else:
            P.op(en, lambda e: e.tensor_scalar(out=out, in0=in0, scalar1=s1, scalar2=s2, op0=op0, op1=op1), reads, writes)

    def STT(en, out, in0, scalar, in1, op0, op1, reads, writes):
        P.op(en, lambda e: e.scalar_tensor_tensor(out=out, in0=in0, scalar=scalar, in1=in1, op0=op0, op1=op1), reads, writes)

    def CP(en, out, in_, reads, writes):
        if en == "act":
            P.op("act", lambda e: e.copy(out, in_), reads, writes)
        else:
            P.op(en, lambda e: e.tensor_copy(out, in_), reads, writes)

    def MEMSET(en, ap, val, writes):
        P.op(en, lambda e: e.memset(ap, val), (), writes)

    def RSQRT(ap, tok_):
        ACT(ap, ap, AF.Sqrt, [tok_], [tok_])
        P.op("dve", lambda e: e.reciprocal(out=ap, in_=ap), [tok_], [tok_])

    def RSUM(en, out, in_, reads, writes):
        P.op(en, lambda e: e.reduce_sum(out=out, in_=in_, axis=AX.X), reads, writes)

    def gather(name, src_ap, shape, dt=F32):
        t = P.tok(name)
        if R == 1:
            return src_ap, t
        n0 = shape[0] // R
        if len(shape) == 3:
            rows, cols = n0 * shape[1], shape[2]
        else:
            rows, cols = n0, shape[1]
        bounce = dscr(name + "_b", [rows, cols], dt)
        full = dscr(name + "_f", [R * rows, cols], dt)
        tbs = []
        if len(shape) == 3:
            for i in range(n0):
                tb = P.tok()
                P.dma("sp", bounce.ap()[i * shape[1]:(i + 1) * shape[1], :], src_ap[i], writes=[tb])
                tbs.append(tb)
            full_ap = full.ap().rearrange("(e k) c -> e k c", k=shape[1])
        else:
            tb = P.tok()
            P.dma("sp", bounce.ap(), src_ap, writes=[tb])
            tbs.append(tb)
            full_ap = full.ap()
        P.collective(lambda e: e.collective_compute("AllGather", ALU.bypass, replica_groups=[list(range(R))],
                                                    ins=[bounce.ap()], outs=[full.ap()]),
                     reads=tbs, writes=[t])
        return full_ap, t

    ident_f = AR.alloc([128, 128], F32); t_identf = P.tok()
    ident_b = AR.alloc([128, 128], BF16); t_identb = P.tok()
    ones_f = AR.alloc([128, 128], F32); t_ones = P.tok()
    modc = AR.alloc([128, L, 96], F32); t_modc = P.tok()
    geff = AR.alloc([128, L, 2, KC], F32); t_geff = P.tok()
    normg = AR.alloc([128, L, 2, KC], F32); t_normg = P.tok()
    finalg = AR.alloc([128, KC], F32); t_finalg = P.tok()
    uT = None
    t_uT = [P.tok("uT%d" % g) for g in range(8)]

    P.dma("sp", ident_f, ident_d.ap(), writes=[t_identf])
    P.dma("sp", ones_f, ones_d.ap(), writes=[t_ones])
    P.dma("sp", normg, normg_d.ap(), writes=[t_normg])
    P.dma("sp", finalg, finalg_d.ap(), writes=[t_finalg])
    CP("dve", ident_b, ident_f, [t_identf], [t_identb])

    m0 = AR.mark()
    c_col = AR.alloc([128, KC, 8], F32); t_c = P.tok()
    scT = AR.alloc([128, KC, 8], BF16); t_sc = P.tok()
    P.dma("sp", c_col, c_col_d.ap(), writes=[t_c])
    ACT(scT, c_col, AF.Silu, [t_c], [t_sc])
    modpart = AR.alloc([128, L * JC * 8], F32); t_mp = P.tok()
    NCOL = 12288 // R
    wblk = [AR.alloc([128, KC, 512], BF16) for _ in range(2)]
    t_wblk = [P.tok() for _ in range(2)]
    nb = 0
    for l in range(L):
        adaw_v = ada_w_d.ap()[l].rearrange("(kc p) c -> p kc c", p=128)
        for jb in range(NCOL // 512):
            wb, tw = wblk[nb % 2], t_wblk[nb % 2]
            nb += 1
            P.dma("pool", wb, adaw_v[:, :, jb * 512:(jb + 1) * 512], writes=[tw])
            b = bank()
            for j in range(4):
                for kc in range(KC):
                    MM(PB[b][:, j * 8:(j + 1) * 8], wb[:, kc, j * 128:(j + 1) * 128], scT[:, kc, :],
                       kc == 0, kc == KC - 1, [tw, t_sc], [PT[b]])
            jc0 = jb * 4
            c0 = (l * JC + jc0) * 8
            CP("dve", modpart[:, c0:c0 + 32], PB[b][:, 0:32], [PT[b]], [t_mp])
    if R > 1:
        ada_in = dscr("ada_in", [128, L * JC * 8])
        ada_all = dscr("ada_all", [R * 128, L * JC * 8])
        t_ai, t_aa = P.tok(), P.tok()
        P.dma("sp", ada_in.ap(), modpart, reads=[t_mp], writes=[t_ai])
        P.collective(lambda e: e.collective_compute("AllGather", ALU.bypass, replica_groups=[list(range(R))],
                                                    ins=[ada_in.ap()], outs=[ada_all.ap()]),
                     reads=[t_ai], writes=[t_aa])
        modall = AR.alloc([128, R, L * JC * 8], F32); t_ma = P.tok()
        P.dma("sp", modall, ada_all.ap().rearrange("(r p) f -> p r f", p=128), reads=[t_aa], writes=[t_ma])
        modall2 = modall.rearrange("p r f -> p (r f)")
    else:
        modall2, t_ma = modpart, t_mp
    onehot = AR.alloc([128, R * L * JC, 8], F32); t_oh = P.tok()
    P.dma("sp", onehot, onehot_d.ap(), writes=[t_oh])
    modsel3 = AR.alloc([128, R * L * JC, 8], F32); t_ms3 = P.tok()
    TT("dve", modsel3, modall2.rearrange("p (a b) -> p a b", b=8), onehot, ALU.mult, [t_ma, t_oh], [t_ms3])
    modsel = AR.alloc([128, R, L, JC], F32); t_ms = P.tok()
    RSUM("dve", modsel.rearrange("p r l j -> p (r l j)"), modsel3, [t_ms3], [t_ms])
    adab = AR.alloc([128, L, 96], F32); t_adab = P.tok()
    P.dma("sp", adab, ada_b_d.ap(), writes=[t_adab])
    for l in range(L):
        TT("dve", modc[:, l, :].rearrange("p (r j) -> p r j", r=R), modsel[:, :, l, :],
           adab[:, l, :].rearrange("p (r j) -> p r j", r=R), ALU.add, [t_ms, t_adab], [t_modc])
    tmp16 = AR.alloc([128, KC], F32); t_tmp16 = P.tok()
    for l in range(L):
        for w in range(2):
            sc = modc[:, l, 16 + 48 * w:32 + 48 * w]
            TS("dve", tmp16, sc, 1.0, None, ALU.add, None, [t_modc], [t_tmp16])
            TT("dve", geff[:, l, w, :], tmp16, normg[:, l, w, :], ALU.mult, [t_tmp16, t_normg], [t_geff])
    P.barrier()
    AR.release(m0)

    def sh_col(l, w):
        return modc[:, l, 48 * w:48 * w + 16]

    def gate_col(l, w):
        return modc[:, l, 32 + 48 * w:48 + 48 * w]

    m0 = AR.mark()
    xt = [AR.alloc([128, D], F32) for _ in range(2)]; t_xt = [P.tok() for _ in range(2)]
    xT = [AR.alloc([128, KC, 128], F32) for _ in range(2)]; t_xT = [P.tok() for _ in range(2)]
    t_hT = [P.tok("hT%d" % g) for g in range(8)]
    for t in range(NT):
        a, ta = xt[t % 2], t_xt[t % 2]
        o, to = xT[t % 2], t_xT[t % 2]
        P.dma("sp", a, x_d.ap()[t * 128:(t + 1) * 128, :], writes=[ta])
        for kq in range(4):
            b = bank()
            for j in range(4):
                kc = kq * 4 + j
                TR(PB[b][:, j * 128:(j + 1) * 128], a[:, kc * 128:(kc + 1) * 128], ident_f, [ta, t_identf], [PT[b]])
            CP("act" if kq % 2 else "dve", o[:, kq * 4:(kq + 1) * 4, :],
               PB[b][:, :].rearrange("p (a b) -> p a b", a=4), [PT[b]], [to])
        P.dma("sp", hT_v[:, :, t * 128:(t + 1) * 128], o, reads=[to], writes=[t_hT[t // 2]])
    P.barrier()
    AR.release(m0)

    def norm_stage(l, w, router=None):
        m = AR.mark()
        hg = [AR.alloc([128, KC, 256], F32) for _ in range(2)]; t_hg = [P.tok() for _ in range(2)]
        sq = [AR.alloc([128, 256], F32) for _ in range(3)]; t_sq = [P.tok() for _ in range(3)]
        rs = [AR.alloc([128, 256], F32) for _ in range(2)]; t_rs = [P.tok() for _ in range(2)]
        tmp = [AR.alloc([128, 256], F32) for _ in range(3)]; t_tmp = [P.tok() for _ in range(3)]
        uf = [AR.alloc([128, 256], F32) for _ in range(3)]; t_uf = [P.tok() for _ in range(3)]
        ge = geff[:, l, w, :]
        sh = sh_col(l, w)
        nsq = 0
        for g in range(8):
            h, th = hg[g % 2], t_hg[g % 2]
            P.dma("sp", h, hT_v[:, :, g * 256:(g + 1) * 256], reads=[t_hT[g]], writes=[th])
            b = bank()
            for kc in range(KC):
                s_, ts_ = sq[nsq % 3], t_sq[nsq % 3]
                nsq += 1
                ACT(s_, h[:, kc, :], AF.Square, [th], [ts_])
                MM(PB[b][:, 0:256], ones_f, s_, kc == 0, kc == KC - 1, [t_ones, ts_], [PT[b]])
            r_, tr_ = rs[g % 2], t_rs[g % 2]
            TS("dve", r_, PB[b][:, 0:256], 1.0 / D, EPS, ALU.mult, ALU.add, [PT[b]], [tr_])
            RSQRT(r_, tr_)
            if router is not None:
                rb = [bank(), bank()]
            for kc in range(KC):
                i3 = (g * KC + kc) % 3
                t_, tt_ = tmp[i3], t_tmp[i3]
                STT("dve", t_, h[:, kc, :], ge[:, kc:kc + 1], r_, ALU.mult, ALU.mult, [th, t_geff, tr_], [tt_])
                if router is None:
                    ACT(uT[:, kc, g * 256:(g + 1) * 256], t_, AF.Identity, [tt_, t_modc], [t_uT[g]],
                        bias=sh[:, kc:kc + 1], scale=1.0)
                else:
                    u_, tu_ = uf[i3], t_uf[i3]
                    ACT(u_, t_, AF.Identity, [tt_, t_modc], [tu_], bias=sh[:, kc:kc + 1], scale=1.0)
                    CP("dve", uT[:, kc, g * 256:(g + 1) * 256], u_, [tu_], [t_uT[g]])
                    for tt in range(2):
                        MM(PB[rb[tt]][:, 0:72], u_[:, tt * 128:(tt + 1) * 128], router["w"][:, kc, :],
                           kc == 0, kc == KC - 1, [tu_, router["tw"]], [PT[rb[tt]]])
            if router is not None:
                for tt in range(2):
                    router["post"](g * 2 + tt, rb[tt])
        P.barrier()
        AR.release(m)

    STG = {"bufs": None, "toks": None, "n": 0, "eng": 0}

    def stage_bufs():
        STG["bufs"] = [AR.alloc([128, KC, 128], F32) for _ in range(2)]
        STG["toks"] = [P.tok() for _ in range(2)]

    def load_w(dst, src_ap, c0, ncols, tw, tsrc):
        v = src_ap.rearrange("(kc p) c -> p kc c", p=128)
        en = "act" if STG["eng"] % 2 else "dve"
        STG["eng"] += 1
        for j0 in range(0, ncols, 128):
            n = min(128, ncols - j0)
            i = STG["n"] % 2
            STG["n"] += 1
            sb_, ts_ = STG["bufs"][i], STG["toks"][i]
            P.dma("sp", sb_[:, :, 0:n], v[:, :, c0 + j0:c0 + j0 + n], reads=[tsrc], writes=[ts_])
            CP(en, dst[:, :, j0:j0 + n], sb_[:, :, 0:n], [ts_], [tw])

    def proj_fm(b, wt, tw, j0, ncol, tg):
        for kc in range(KC):
            MM(PB[b][0:ncol, :], wt[:, kc, j0:j0 + ncol], uT[:, kc, tg * 512:(tg + 1) * 512],
               kc == 0, kc == KC - 1, [tw, t_uT[2 * tg], t_uT[2 * tg + 1]], [PT[b]])

    def lru_stage(l, w_in_ap, t_win):
        m = AR.mark()
        stage_bufs()
        lcol = AR.alloc([128, 4, 8], F32); t_lcol = P.tok()
        P.dma("sp", lcol, lru_col_d.ap()[:, l, :, :], writes=[t_lcol])
        cl = AR.alloc([128, 4, 2], F32); t_cl = P.tok()
        e1 = AR.alloc([128, 4], F32); t_e1 = P.tok()
        ACT(e1, lcol[:, :, 7], AF.Exp, [t_lcol], [t_e1], scale=-1.0)
        ACT(e1, e1, AF.Ln, [t_e1], [t_e1], bias=1.0, scale=1.0)
        TS("dve", cl[:, :, 0], e1, -8.0, None, ALU.mult, None, [t_e1], [t_cl])
        TS("dve", cl[:, :, 1], e1, -16.0, None, ALU.mult, None, [t_e1], [t_cl])
        wy = [AR.alloc([128, KC, 128], BF16) for _ in range(2)]; t_wy = [P.tok() for _ in range(2)]
        wx = [AR.alloc([128, KC, 128], BF16) for _ in range(2)]; t_wx = [P.tok() for _ in range(2)]
        wbd = [AR.alloc([128, 2, 128], F32) for _ in range(2)]; t_wbd = [P.tok() for _ in range(2)]
        xpad = AR.alloc([128, 3 + S], F32); t_xpad = P.tok()
        xc = AR.alloc([128, S], F32); t_xc = P.tok()
        r_ = AR.alloc([128, S], F32); t_r = P.tok()
        ig = AR.alloc([128, S], F32); t_ig = P.tok()
        a_ = AR.alloc([128, S], F32); t_a = P.tok()
        b_ = AR.alloc([128, S], F32); t_b = P.tok()
        yv = AR.alloc([128, S], F32); t_yv = P.tok()
        y2 = AR.alloc([128, S], F32); t_y2 = P.tok()
        ob = [AR.alloc([128, S], BF16) for _ in range(2)]; t_ob = [P.tok() for _ in range(2)]
        MEMSET("dve", xpad[:, 0:3], 0.0, [t_xpad])
        for cc in range(4):
            i2 = cc % 2
            load_w(wx[i2], w_in_ap, OFF_LX + cc * 128, 128, t_wx[i2], t_win)
            load_w(wy[i2], w_in_ap, OFF_LY + cc * 128, 128, t_wy[i2], t_win)
            P.dma("sp", wbd[i2], lru_wbd_d.ap()[l, cc].rearrange("w k m -> k w m"), writes=[t_wbd[i2]])
            for tg in range(4):
                b = bank()
                proj_fm(b, wx[i2], t_wx[i2], 0, 128, tg)
                CP("act", xpad[:, 3 + tg * 512:3 + (tg + 1) * 512], PB[b][:, :], [PT[b]], [t_xpad])
            TS("dve", xc, xpad[:, 0:S], lcol[:, cc, 0:1], lcol[:, cc, 4:5], ALU.mult, ALU.add, [t_xpad, t_lcol], [t_xc])
            for i in range(1, 4):
                STT("dve", xc, xpad[:, i:i + S], lcol[:, cc, i:i + 1], xc, ALU.mult, ALU.add, [t_xpad, t_lcol, t_xc], [t_xc])
            for tg in range(4):
                b = bank()
                MM(PB[b][:, :], wbd[i2][:, 0, :], xc[:, tg * 512:(tg + 1) * 512], True, True, [t_wbd[i2], t_xc], [PT[b]])
                ACT(r_[:, tg * 512:(tg + 1) * 512], PB[b][:, :], AF.Sigmoid, [PT[b], t_lcol], [t_r], bias=lcol[:, cc, 5:6], scale=1.0)
                b = bank()
                MM(PB[b][:, :], wbd[i2][:, 1, :], xc[:, tg * 512:(tg + 1) * 512], True, True, [t_wbd[i2], t_xc], [PT[b]])
                ACT(ig[:, tg * 512:(tg + 1) * 512], PB[b][:, :], AF.Sigmoid, [PT[b], t_lcol], [t_ig], bias=lcol[:, cc, 6:7], scale=1.0)
            ACT(a_, r_, AF.Exp, [t_r, t_cl], [t_a], scale=cl[:, cc, 0:1])
            ACT(b_, r_, AF.Exp, [t_r, t_cl], [t_b], scale=cl[:, cc, 1:2])
            TS("dve", b_, b_, -1.0, 1.0, ALU.mult, ALU.add, [t_b], [t_b])
            ACT(b_, b_, AF.Sqrt, [t_b], [t_b])
            TT("dve", ig, ig, xc, ALU.mult, [t_ig, t_xc], [t_ig])
            TT("dve", b_, b_, ig, ALU.mult, [t_b, t_ig], [t_b])
            P.op("dve", lambda e, a_=a_, b_=b_, r_=r_: e.tensor_tensor_scan(out=r_, data0=a_, data1=b_, initial=0.0,
                                                                         op0=ALU.mult, op1=ALU.add),
                 [t_a, t_b], [t_r])
            for tg in range(4):
                b = bank()
                proj_fm(b, wy[i2], t_wy[i2], 0, 128, tg)
                CP("act", yv[:, tg * 512:(tg + 1) * 512], PB[b][:, :], [PT[b]], [t_yv])
            ACT(y2, yv, AF.Square, [t_yv], [t_y2])
            TS("dve", y2, y2, 0.044715, 1.0, ALU.mult, ALU.add, [t_y2], [t_y2])
            TT("dve", y2, y2, yv, ALU.mult, [t_y2, t_yv], [t_y2])
            ACT(y2, y2, AF.Sigmoid, [t_y2], [t_y2], scale=1.5957691216057308)
            TT("dve", y2, y2, yv, ALU.mult, [t_y2, t_yv], [t_y2])
            TT("dve", ob[i2], r_, y2, ALU.mult, [t_r, t_y2], [t_ob[i2]])
            P.dma("sp", mixT_v[:, 6 + cc, :], ob[i2], reads=[t_ob[i2]], writes=[t_mix])
        P.barrier()
        AR.release(m)


    def gla_stage(l, w_in_ap, t_win):
        m = AR.mark()
        stage_bufs()
        tri = AR.alloc([64, 3, 64], F32); t_tri = P.tok()
        P.dma("sp", tri, tri_d.ap()[:, 0:64, 0:64].rearrange("a k m -> k a m"), writes=[t_tri])
        gmask = AR.alloc([64, 2, 192], F32); t_gmask = P.tok()
        P.dma("sp", gmask, gmask_d.ap().rearrange("a k m -> k a m"), writes=[t_gmask])
        wa2 = AR.alloc([17, 384], F32); t_wa2 = P.tok()
        P.dma("sp", wa2, gla_wa2_d.ap()[l], writes=[t_wa2])
        gn = AR.alloc([64, 768], F32); t_gn = P.tok()
        P.dma("sp", gn, gla_ng_d.ap()[l:l + 1, :].partition_broadcast(64), writes=[t_gn])
        wga = AR.alloc([128, KC, 16], BF16); t_wga = P.tok()
        load_w(wga, w_in_ap, OFF_GA, 16, t_wga, t_win)
        gaT = AR.alloc([17, S], F32); t_gaT = P.tok()
        MEMSET("dve", gaT, 1.0, [t_gaT])
        for tg in range(4):
            b = bank()
            proj_fm(b, wga, t_wga, 0, 16, tg)
            CP("act", gaT[0:16, tg * 512:(tg + 1) * 512], PB[b][0:16, :], [PT[b]], [t_gaT])
        wq = AR.alloc([128, KC, 192], BF16); t_wq = P.tok()
        wk = AR.alloc([128, KC, 192], BF16); t_wk = P.tok()
        wv = AR.alloc([128, KC, 384], BF16); t_wv = P.tok()
        wog = AR.alloc([128, KC, 384], BF16); t_wog = P.tok()
        S_ = AR.alloc([64, 3, 128], F32); t_S = P.tok()
        Sb = AR.alloc([64, 3, 128], BF16); t_Sb = P.tok()
        mixg = AR.alloc([128, 3, S], BF16); t_mixg = P.tok()

        def buf2(shape, dt):
            return [AR.alloc(shape, dt) for _ in range(2)], [P.tok() for _ in range(2)]
        zA, t_zA = buf2([64, 192], F32)
        zB, t_zB = buf2([64, 192], F32)
        la, t_la = buf2([64, 192], F32)
        eG, t_eG = buf2([64, 192], F32)
        enG, t_enG = buf2([64, 192], F32)
        eR, t_eR = buf2([64, 192], F32)
        dec, t_dec = buf2([64, 3], F32)
        V, t_V = buf2([64, 384], BF16)
        SG, t_SG = buf2([64, 384], F32)
        KD, t_KD = buf2([64, 192], BF16)
        X4, t_X4 = buf2([64, 4, 192], BF16)
        XT, t_XT = buf2([64, 768], BF16)
        s1, t_s1 = buf2([64, 192], F32)
        s2, t_s2 = buf2([64, 192], F32)
        at, t_at = buf2([64, 192], BF16)
        sq, t_sq = buf2([64, 384], F32)
        ss, t_ss = buf2([64, 3], F32)
        y, t_y = buf2([64, 384], F32)
        yb, t_yb = buf2([64, 384], BF16)
        idb = ident_b[0:64, 0:64]
        for hg in range(GDBG["nhp"] if GDBG["nhp"] < 3 else 2):
            load_w(wq, w_in_ap, OFF_GQ + hg * 192, 192, t_wq, t_win)
            load_w(wk, w_in_ap, OFF_GK + hg * 192, 192, t_wk, t_win)
            load_w(wv, w_in_ap, OFF_GV + hg * 384, 384, t_wv, t_win)
            load_w(wog, w_in_ap, OFF_GOG + hg * 384, 384, t_wog, t_win)
            MEMSET("dve", S_, 0.0, [t_S])
            MEMSET("dve", Sb, 0.0, [t_Sb])
            for n in range(2 * GDBG["nt"]):
                j2 = n % 2
                tk = slice(n * 64, (n + 1) * 64)
                tu = [t_uT[n // 4]]
                b = bank()
                MM(PB[b][0:64, 0:192], gaT[0:17, tk], wa2[0:17, hg * 192:(hg + 1) * 192], True, True, [t_gaT, t_wa2], [PT[b]])
                ACT(zA[j2], PB[b][0:64, 0:192], AF.Relu, [PT[b]], [t_zA[j2]], scale=-1.0)
                ACT(zB[j2], PB[b][0:64, 0:192], AF.Abs, [PT[b]], [t_zB[j2]])
                ACT(zB[j2], zB[j2], AF.Exp, [t_zB[j2]], [t_zB[j2]], scale=-1.0)
                ACT(zB[j2], zB[j2], AF.Ln, [t_zB[j2]], [t_zB[j2]], bias=1.0, scale=1.0)
                STT("dve", la[j2], zB[j2], -1.0, zA[j2], ALU.mult, ALU.subtract, [t_zA[j2], t_zB[j2]], [t_la[j2]])
                bq = bank()
                for kc in range(KC):
                    MM(PB[bq][0:64, 0:192], uT[:, kc, tk], wq[:, kc, :], kc == 0, kc == KC - 1, tu + [t_wq], [PT[bq]])
                for kc in range(KC):
                    MM(PB[bq][0:64, 256:448], uT[:, kc, tk], wk[:, kc, :], kc == 0, kc == KC - 1, tu + [t_wk], [PT[bq]])
                bv = bank()
                for kc in range(KC):
                    MM(PB[bv][0:64, 0:384], uT[:, kc, tk], wv[:, kc, :], kc == 0, kc == KC - 1, tu + [t_wv], [PT[bv]])
                bo = bank()
                for kc in range(KC):
                    MM(PB[bo][0:64, 0:384], uT[:, kc, tk], wog[:, kc, :], kc == 0, kc == KC - 1, tu + [t_wog], [PT[bo]])
                bg = bank()
                MM(PB[bg][0:64, 0:192], tri[:, 0, :], la[j2], True, True, [t_tri, t_la[j2]], [PT[bg]])
                MM(PB[bg][0:64, 256:448], tri[:, 1, :], la[j2], True, True, [t_tri, t_la[j2]], [PT[bg]])
                for hh in range(3):
                    MM(PB[bg][0:64, 480 + hh:481 + hh], la[j2][:, hh * 64:(hh + 1) * 64], tri[:, 2, 0:1], True, True,
                       [t_tri, t_la[j2]], [PT[bg]])
                ACT(eG[j2], PB[bg][0:64, 0:192], AF.Exp, [PT[bg]], [t_eG[j2]])
                ACT(enG[j2], PB[bg][0:64, 0:192], AF.Exp, [PT[bg]], [t_enG[j2]], scale=-1.0)
                ACT(eR[j2], PB[bg][0:64, 256:448], AF.Exp, [PT[bg]], [t_eR[j2]])
                ACT(dec[j2], PB[bg][0:64, 480:483], AF.Exp, [PT[bg]], [t_dec[j2]])
                CP("act", V[j2], PB[bv][0:64, 0:384], [PT[bv]], [t_V[j2]])
                ACT(SG[j2], PB[bo][0:64, 0:384], AF.Silu, [PT[bo]], [t_SG[j2]])
                TT("dve", KD[j2], PB[bq][0:64, 256:448], eR[j2], ALU.mult, [PT[bq], t_eR[j2]], [t_KD[j2]])
                STT("dve", X4[j2][:, 0, :], PB[bq][0:64, 0:192], 0.125, eG[j2], ALU.mult, ALU.mult, [PT[bq], t_eG[j2]], [t_X4[j2]])
                TT("dve", X4[j2][:, 1, :], PB[bq][0:64, 256:448], enG[j2], ALU.mult, [PT[bq], t_enG[j2]], [t_X4[j2]])
                STT("dve", X4[j2][:, 2, :], PB[bq][0:64, 0:192], 0.125, enG[j2], ALU.mult, ALU.mult, [PT[bq], t_enG[j2]], [t_X4[j2]])
                TT("dve", X4[j2][:, 3, :], PB[bq][0:64, 256:448], eG[j2], ALU.mult, [PT[bq], t_eG[j2]], [t_X4[j2]])
                pbb, tpb = tbank()
                for w in range(4):
                    for hh in range(3):
                        c0 = (w * 3 + hh) * 64
                        TR(pbb[0:64, c0:c0 + 64], X4[j2][:, w, hh * 64:(hh + 1) * 64], idb, [t_X4[j2], t_identb], [tpb])
                CP("act", XT[j2], pbb[0:64, 0:768], [tpb], [t_XT[j2]])

                def xt(w, hh):
                    c0 = (w * 3 + hh) * 64
                    return XT[j2][:, c0:c0 + 64]
                bs = bank()
                for hh in range(3):
                    MM(PB[bs][0:64, hh * 64:(hh + 1) * 64], xt(1, hh), xt(0, hh), True, True, [t_XT[j2]], [PT[bs]])
                    MM(PB[bs][0:64, 256 + hh * 64:256 + (hh + 1) * 64], xt(3, hh), xt(2, hh), True, True, [t_XT[j2]], [PT[bs]])
                TT("dve", s1[j2], PB[bs][0:64, 0:192], gmask[:, 0, :], ALU.mult, [PT[bs], t_gmask], [t_s1[j2]])
                TT("dve", s2[j2], PB[bs][0:64, 256:448], gmask[:, 1, :], ALU.mult, [PT[bs], t_gmask], [t_s2[j2]])
                TT("dve", at[j2], s1[j2], s2[j2], ALU.add, [t_s1[j2], t_s2[j2]], [t_at[j2]])
                bo2 = bank()
                bkv = bank()
                for hh in range(3):
                    o_ap = PB[bo2][0:64, hh * 128:(hh + 1) * 128]
                    MM(o_ap, at[j2][:, hh * 64:(hh + 1) * 64], V[j2][:, hh * 128:(hh + 1) * 128], True, False, [t_at[j2], t_V[j2]], [PT[bo2]])
                    MM(o_ap, xt(0, hh), Sb[:, hh, :], False, True, [t_XT[j2], t_Sb], [PT[bo2]])
                for hh in range(3):
                    MM(PB[bkv][0:64, hh * 128:(hh + 1) * 128], KD[j2][:, hh * 64:(hh + 1) * 64], V[j2][:, hh * 128:(hh + 1) * 128], True, True,
                       [t_KD[j2], t_V[j2]], [PT[bkv]])
                for hh in range(3):
                    STT("dve", S_[:, hh, :], S_[:, hh, :], dec[j2][:, hh:hh + 1], PB[bkv][0:64, hh * 128:(hh + 1) * 128], ALU.mult, ALU.add,
                        [t_S, t_dec[j2], PT[bkv]], [t_S])
                CP("act", Sb, S_, [t_S], [t_Sb])
                ACT(sq[j2], PB[bo2][0:64, 0:384], AF.Square, [PT[bo2]], [t_sq[j2]])
                RSUM("dve", ss[j2], sq[j2].rearrange("p (a b) -> p a b", a=3), [t_sq[j2]], [t_ss[j2]])
                TS("dve", ss[j2], ss[j2], 1.0 / 128, EPS, ALU.mult, ALU.add, [t_ss[j2]], [t_ss[j2]])
                RSQRT(ss[j2], t_ss[j2])
                for hh in range(3):
                    TS("dve", y[j2][:, hh * 128:(hh + 1) * 128], PB[bo2][0:64, hh * 128:(hh + 1) * 128], ss[j2][:, hh:hh + 1], None,
                       ALU.mult, None, [PT[bo2], t_ss[j2]], [t_y[j2]])
                TT("dve", y[j2], y[j2], gn[:, hg * 384:(hg + 1) * 384], ALU.mult, [t_y[j2], t_gn], [t_y[j2]])
                TT("dve", yb[j2], y[j2], SG[j2], ALU.mult, [t_y[j2], t_SG[j2]], [t_yb[j2]])
                pbb, tpb = tbank()
                for hh in range(3):
                    TR(pbb[:, hh * 64:(hh + 1) * 64], yb[j2][:, hh * 128:(hh + 1) * 128], idb, [t_yb[j2], t_identb], [tpb])
                CP("act", mixg[:, :, tk], pbb[:, 0:192].rearrange("p (a b) -> p a b", a=3), [tpb], [t_mixg])
            P.dma("sp", mixT_v[:, 3 * hg:3 * hg + 3, :], mixg, reads=[t_mixg], writes=[t_mix])
        P.barrier()
        AR.release(m)

    def diff_stage(l, w_in_ap, t_win):
        m = AR.mark()
        DST = GDBG.get("dstop", 99)
        stage_bufs()
        lam_init = 0.8 - 0.6 * math.exp(-0.3 * l)
        relb = AR.alloc([128, 192], F32); t_relb = P.tok()
        P.dma("sp", relb, relb_d.ap().partition_broadcast(128), writes=[t_relb])
        BT = AR.alloc([128, 6, 256], F32); t_BT = P.tok()
        m2 = AR.mark()
        oh = AR.alloc([128, 32, 256], F32); t_ohh = P.tok()
        P.dma("sp", oh, t5oh_d.ap(), writes=[t_ohh])
        tmask = AR.alloc([128, 256], F32); t_tmask = P.tok()
        P.dma("sp", tmask, t5mask_d.ap(), writes=[t_tmask])
        for h in range(6):
            TS("dve", BT[:, h, :], oh[:, 0, :], relb[:, h:h + 1], None, ALU.mult, None, [t_ohh, t_relb], [t_BT])
            for bq in range(1, 32):
                STT("dve", BT[:, h, :], oh[:, bq, :], relb[:, bq * 6 + h:bq * 6 + h + 1], BT[:, h, :], ALU.mult, ALU.add,
                    [t_ohh, t_relb, t_BT], [t_BT])
            TT("dve", BT[:, h, :], BT[:, h, :], tmask, ALU.add, [t_BT, t_tmask], [t_BT])
        P.barrier()
        AR.release(m2)
        dl = AR.alloc([128, 256], F32); t_dl = P.tok()
        P.dma("sp", dl, diff_l_d.ap()[l:l + 1, :].partition_broadcast(128), writes=[t_dl])
        pr = AR.alloc([128, 128], F32); t_pr = P.tok()
        TT("dve", pr[:, 0:64], dl[:, 0:64], dl[:, 64:128], ALU.mult, [t_dl], [t_pr])
        TT("dve", pr[:, 64:128], dl[:, 128:192], dl[:, 192:256], ALU.mult, [t_dl], [t_pr])
        e2 = AR.alloc([128, 2], F32); t_e2 = P.tok()
        RSUM("dve", e2, pr.rearrange("p (a b) -> p a b", a=2), [t_pr], [t_e2])
        ACT(e2, e2, AF.Exp, [t_e2], [t_e2])
        nlam = AR.alloc([128, 1], F32); t_nlam = P.tok()
        TT("dve", nlam, e2[:, 1:2], e2[:, 0:1], ALU.subtract, [t_e2], [t_nlam])
        TS("dve", nlam, nlam, -lam_init, None, ALU.add, None, [t_nlam], [t_nlam])
        sg = AR.alloc([128, 128], F32); t_sg = P.tok()
        P.dma("sp", sg, subln_d.ap()[l:l + 1, :].partition_broadcast(128), writes=[t_sg])
        TS("dve", sg, sg, 1.0 - lam_init, None, ALU.mult, None, [t_sg], [t_sg])

        if DST <= 1:
            dbg_out["dbg_bt"] = nc.dram_tensor("dbg_bt", [128, 6, 256], F32, kind="ExternalOutput")
            P.dma("sp", dbg_out["dbg_bt"].ap(), BT, reads=[t_BT])
            P.barrier()
            AR.release(m)
            return

        def buf2(shape, dt, n=2):
            return [AR.alloc(shape, dt) for _ in range(n)], [P.tok() for _ in range(n)]
        wq, t_wq = buf2([128, KC, 128], BF16)
        wk, t_wk = buf2([128, KC, 128], BF16)
        wv, t_wv = buf2([128, KC, 128], BF16)
        QT, t_QT = buf2([64, 2, S], BF16)
        KT, t_KT = buf2([64, 2, S], BF16)
        VE, t_VE = buf2([128, NT, 144], BF16)
        mixd, t_mixd = buf2([128, S], BF16)
        PTb, t_PTb = buf2([128, 512], BF16, 3)
        scb, t_scb = buf2([128, 128], F32, 3)
        rr, t_rr = buf2([128, 2], F32)
        dd, t_dd = buf2([128, 128], F32)
        sq, t_sq = buf2([128, 128], F32)
        ss, t_ss = buf2([128, 1], F32)
        yb, t_yb = buf2([128, 128], BF16)
        npt = 0
        nsc = 0
        nrot[0] = 4
        for h in range(GDBG.get("dnh", 6)):
            i2 = h % 2
            load_w(wq[i2], w_in_ap, OFF_DQ + h * 128, 128, t_wq[i2], t_win)
            load_w(wk[i2], w_in_ap, OFF_DK + h * 128, 128, t_wk[i2], t_win)
            load_w(wv[i2], w_in_ap, OFF_DV + h * 128, 128, t_wv[i2], t_win)
            MEMSET("dve", VE[i2].rearrange("p a b -> p (a b)"), 1.0, [t_VE[i2]])
            for tg in range(4):
                for mm_ in range(2):
                    b = bank()
                    proj_fm(b, wq[i2], t_wq[i2], mm_ * 64, 64, tg)
                    ACT(QT[i2][:, mm_, tg * 512:(tg + 1) * 512], PB[b][0:64, :], AF.Identity, [PT[b]], [t_QT[i2]], scale=0.125)
                    b = bank()
                    proj_fm(b, wk[i2], t_wk[i2], mm_ * 64, 64, tg)
                    CP("dve", KT[i2][:, mm_, tg * 512:(tg + 1) * 512], PB[b][0:64, :], [PT[b]], [t_KT[i2]])
            for t in range(NT):
                b = bank()
                for kc in range(KC):
                    MM(PB[b][:, 0:128], uT[:, kc, t * 128:(t + 1) * 128], wv[i2][:, kc, :], kc == 0, kc == KC - 1,
                       [t_uT[t // 2], t_wv[i2]], [PT[b]])
                CP("act", VE[i2][:, t, 0:128], PB[b][:, 0:128], [PT[b]], [t_VE[i2]])
            for qt in range(GDBG.get("dnq", NT) if DST > 2 else 0):
                j2 = qt % 2
                ql = slice(qt * 128, (qt + 1) * 128)
                bO = 4 + (qt % 2)
                nk = qt + 1
                for mm_ in range(2):
                    rw = slice(mm_ * 64, mm_ * 64 + 64)
                    for g0 in range(0, nk, 4):
                        kts = list(range(g0, min(g0 + 4, nk)))
                        bs = bank()
                        for i, kt in enumerate(kts):
                            MM(PB[bs][:, i * 128:(i + 1) * 128], KT[i2][:, mm_, kt * 128:(kt + 1) * 128], QT[i2][:, mm_, ql], True, True,
                               [t_KT[i2], t_QT[i2]], [PT[bs]])
                        pt, tpt = PTb[npt % 3], t_PTb[npt % 3]
                        npt += 1
                        nfar = len([kt for kt in kts if kt <= qt - 2])
                        if nfar:
                            ACT(pt[:, 0:nfar * 128], PB[bs][:, 0:nfar * 128], AF.Exp, [PT[bs], t_relb], [tpt],
                                bias=relb[:, 15 * 6 + h:15 * 6 + h + 1], scale=1.0)
                        for i, kt in enumerate(kts):
                            if kt >= qt - 1:
                                j0 = 0 if kt == qt else 128
                                sc_, tsc_ = scb[nsc % 3], t_scb[nsc % 3]
                                nsc += 1
                                TT("dve", sc_, PB[bs][:, i * 128:(i + 1) * 128], BT[:, h, j0:j0 + 128], ALU.add, [PT[bs], t_BT], [tsc_])
                                ACT(pt[:, i * 128:(i + 1) * 128], sc_, AF.Exp, [tsc_], [tpt])
                        for i, kt in enumerate(kts if DST > 3 else []):
                            MM(PB[bO][:, mm_ * 256:mm_ * 256 + 130], pt[:, i * 128:(i + 1) * 128], VE[i2][:, kt, 0:130], kt == 0, kt == qt,
                               [tpt, t_VE[i2]], [PT[bO]])
                if DST <= 4:
                    continue
                P.op("dve", lambda e, o=rr[j2][:, 0:1], i=PB[bO][:, 128:129]: e.reciprocal(out=o, in_=i), [PT[bO]], [t_rr[j2]])
                P.op("dve", lambda e, o=rr[j2][:, 1:2], i=PB[bO][:, 384:385]: e.reciprocal(out=o, in_=i), [PT[bO]], [t_rr[j2]])
                TT("dve", rr[j2][:, 1:2], rr[j2][:, 1:2], nlam, ALU.mult, [t_rr[j2], t_nlam], [t_rr[j2]])
                TS("dve", dd[j2], PB[bO][:, 0:128], rr[j2][:, 0:1], None, ALU.mult, None, [PT[bO], t_rr[j2]], [t_dd[j2]])
                STT("dve", dd[j2], PB[bO][:, 256:384], rr[j2][:, 1:2], dd[j2], ALU.mult, ALU.add, [PT[bO], t_rr[j2], t_dd[j2]], [t_dd[j2]])
                ACT(sq[j2], dd[j2], AF.Square, [t_dd[j2]], [t_sq[j2]])
                RSUM("dve", ss[j2], sq[j2], [t_sq[j2]], [t_ss[j2]])
                TS("dve", ss[j2], ss[j2], 1.0 / 128, EPS, ALU.mult, ALU.add, [t_ss[j2]], [t_ss[j2]])
                RSQRT(ss[j2], t_ss[j2])
                STT("dve", yb[j2], dd[j2], ss[j2][:, 0:1], sg, ALU.mult, ALU.mult, [t_dd[j2], t_ss[j2], t_sg], [t_yb[j2]])
                pbb, tpb = tbank()
                TR(pbb[:, 0:128], yb[j2], ident_b, [t_yb[j2], t_identb], [tpb])
                CP("act", mixd[i2][:, ql], pbb[:, 0:128], [tpb], [t_mixd[i2]])
            P.dma("sp", mixT_v[:, 10 + h, :], mixd[i2], reads=[t_mixd[i2]], writes=[t_mix])
        nrot[0] = NFB
        P.barrier()
        AR.release(m)

    def make_router(l, wcT, t_wcT):
        rw = AR.alloc([128, KC, 72], F32); t_rw = P.tok()
        P.dma("sp", rw, router_w_d.ap()[l].rearrange("(kc p) c -> p kc c", p=128), writes=[t_rw])
        rbias = AR.alloc([128, 72], F32); t_rb = P.tok()
        P.dma("sp", rbias, router_b_d.ap()[l:l + 1, :].partition_broadcast(128), writes=[t_rb])

        def buf2(shape, dt):
            return [AR.alloc(shape, dt) for _ in range(2)], [P.tok() for _ in range(2)]
        lg, t_lg = buf2([128, 72], F32)
        g8, t_g8 = buf2([128, 8], F32)
        sm, t_sm = buf2([128, 16], F32)
        ex, t_ex = buf2([128, 8], F32)
        ohg, t_ohg = buf2([128, 8], F32)
        lem, t_lem = buf2([128, 64], F32)
        e8, t_e8 = buf2([128, 8], F32)
        m1, t_m1 = buf2([128, 64], F32)
        m2, t_m2 = buf2([128, 64], F32)

        def post(t, rb):
            j = t % 2
            TT("dve", lg[j], PB[rb][:, 0:72], rbias, ALU.add, [PT[rb], t_rb], [t_lg[j]])
            P.op("dve", lambda e, o=g8[j], i=lg[j][:, 0:8]: e.max(out=o, in_=i), [t_lg[j]], [t_g8[j]])
            s_ = sm[j]
            ts_ = t_sm[j]
            TS("dve", s_[:, 0:1], g8[j][:, 0:1], -1.0, None, ALU.mult, None, [t_g8[j]], [ts_])
            ACT(ex[j], lg[j][:, 0:8], AF.Exp, [t_lg[j], ts_], [t_ex[j]], bias=s_[:, 0:1], scale=1.0)
            RSUM("dve", s_[:, 1:2], ex[j], [t_ex[j]], [ts_])
            P.op("dve", lambda e, o=s_[:, 2:3], i=s_[:, 1:2]: e.reciprocal(out=o, in_=i), [ts_], [ts_])
            TS("dve", ohg[j], lg[j][:, 0:8], g8[j][:, 0:1], None, ALU.is_equal, None, [t_lg[j], t_g8[j]], [t_ohg[j]])
            TS("dve", ohg[j], ohg[j], 1e9, -1e9, ALU.mult, ALU.add, [t_ohg[j]], [t_ohg[j]])
            for g in range(8):
                TS("dve", lem[j][:, g * 8:(g + 1) * 8], lg[j][:, 8 + g * 8:16 + g * 8], ohg[j][:, g:g + 1], None, ALU.add, None,
                   [t_lg[j], t_ohg[j]], [t_lem[j]])
            P.op("dve", lambda e, o=e8[j], i=lem[j]: e.max(out=o, in_=i), [t_lem[j]], [t_e8[j]])
            TT("dve", s_[:, 3:4], e8[j][:, 1:2], e8[j][:, 0:1], ALU.subtract, [t_e8[j]], [ts_])
            ACT(s_[:, 4:5], s_[:, 3:4], AF.Sigmoid, [ts_], [ts_], scale=-1.0)
            ACT(s_[:, 5:6], s_[:, 3:4], AF.Sigmoid, [ts_], [ts_])
            TT("dve", s_[:, 6:7], s_[:, 4:5], s_[:, 2:3], ALU.mult, [ts_], [ts_])
            TT("dve", s_[:, 7:8], s_[:, 5:6], s_[:, 2:3], ALU.mult, [ts_], [ts_])
            TS("dve", m1[j], lem[j], e8[j][:, 0:1], s_[:, 6:7], ALU.is_equal, ALU.mult, [t_lem[j], t_e8[j], ts_], [t_m1[j]])
            TS("dve", m2[j], lem[j], e8[j][:, 1:2], s_[:, 7:8], ALU.is_equal, ALU.mult, [t_lem[j], t_e8[j], ts_], [t_m2[j]])
            TT("dve", m1[j], m1[j], m2[j], ALU.add, [t_m1[j], t_m2[j]], [t_m1[j]])
            b = bank()
            TR(PB[b][0:64, 0:128], m1[j], ident_f, [t_m1[j], t_identf], [PT[b]])
            CP("act", wcT[:, t * 128:(t + 1) * 128], PB[b][0:64, 0:128], [PT[b]], [t_wcT])
        return {"w": rw, "tw": t_rw, "post": post}

    def moe_stage(l, w1_ap, w3_ap, w2_ap, t_w1, t_w3, t_w2, wcT, t_wcT):
        m = AR.mark()
        gc = gate_col(l, 1)
        u2h = AR.alloc([128, KC, 1024], BF16); t_u2h = P.tok()
        acc = AR.alloc([128, KC, 1024], F32); t_acc = [P.tok() for _ in range(KC)]

        def bufn(shape, dt, n=2):
            return [AR.alloc(shape, dt) for _ in range(n)], [P.tok() for _ in range(n)]
        w1q, t_w1q = bufn([128, KC, 128], BF16)
        w3q, t_w3q = bufn([128, KC, 128], BF16)
        w2b, t_w2b = bufn([128, 4, D], BF16)
        hidT, t_hid = bufn([128, 4, 1024], BF16)
        wcb, t_wcb = bufn([128, 1024], F32)
        wce = AR.alloc([64, 1024], F32); t_wce = P.tok()
        sl, t_sl = bufn([128, 512], F32)
        t2, t_t2 = bufn([128, 512], F32)
        nq = 0
        ns = 0
        NE = GDBG.get("ne", NEXP)
        for th in range(2):
            tsl = slice(th * 1024, (th + 1) * 1024)
            P.dma("sp", u2h, u2T_v[:, :, tsl], reads=[t_u2T], writes=[t_u2h])
            for q4 in range(4):
                P.dma("sp", acc[:, q4 * 4:(q4 + 1) * 4, :], hT_v[:, q4 * 4:(q4 + 1) * 4, tsl],
                      reads=t_hT[4 * th:4 * th + 4], writes=t_acc[q4 * 4:(q4 + 1) * 4])
            for e in range(NE):
                i = e % 2
                TS("dve", wce, wcT[:, tsl], ident_f[0:64, e:e + 1], None, ALU.mult, None, [t_wcT, t_identf], [t_wce])
                for tg in range(2):
                    b = bank()
                    MM(PB[b][:, :], ones_f[0:64, :], wce[:, tg * 512:(tg + 1) * 512], True, True, [t_ones, t_wce], [PT[b]])
                    CP("act", wcb[i][:, tg * 512:(tg + 1) * 512], PB[b][:, :], [PT[b]], [t_wcb[i]])
                P.dma("pool", w2b[i], w2_ap[e].rearrange("(fc p) d -> p fc d", p=128), reads=[t_w2], writes=[t_w2b[i]])
                w1v = w1_ap[e].rearrange("(kc p) f -> p kc f", p=128)
                w3v = w3_ap[e].rearrange("(kc p) f -> p kc f", p=128)
                for fq in range(4):
                    jq = nq % 2
                    nq += 1
                    P.dma("pool", w1q[jq], w1v[:, :, fq * 128:(fq + 1) * 128], reads=[t_w1], writes=[t_w1q[jq]])
                    P.dma("pool", w3q[jq], w3v[:, :, fq * 128:(fq + 1) * 128], reads=[t_w3], writes=[t_w3q[jq]])
                    for tg in range(2):
                        js = ns % 2
                        ns += 1
                        b1 = bank()
                        for kc in range(KC):
                            MM(PB[b1][:, :], w1q[jq][:, kc, :], u2h[:, kc, tg * 512:(tg + 1) * 512], kc == 0, kc == KC - 1,
                               [t_w1q[jq], t_u2h], [PT[b1]])
                        b3 = bank()
                        for kc in range(KC):
                            MM(PB[b3][:, :], w3q[jq][:, kc, :], u2h[:, kc, tg * 512:(tg + 1) * 512], kc == 0, kc == KC - 1,
                               [t_w3q[jq], t_u2h], [PT[b3]])
                        ACT(sl[js], PB[b1][:, :], AF.Silu, [PT[b1]], [t_sl[js]])
                        TT("dve", t2[js], PB[b3][:, :], wcb[i][:, tg * 512:(tg + 1) * 512], ALU.mult, [PT[b3], t_wcb[i]], [t_t2[js]])
                        TT("dve", hidT[i][:, fq, tg * 512:(tg + 1) * 512], sl[js], t2[js], ALU.mult, [t_sl[js], t_t2[js]], [t_hid[i]])
                for dch in range(KC):
                    for tg in range(2):
                        by = bank()
                        for fc in range(4):
                            MM(PB[by][:, :], w2b[i][:, fc, dch * 128:(dch + 1) * 128], hidT[i][:, fc, tg * 512:(tg + 1) * 512],
                               fc == 0, fc == 3, [t_w2b[i], t_hid[i]], [PT[by]])
                        a_ = acc[:, dch, tg * 512:(tg + 1) * 512]
                        STT("dve", a_, PB[by][:, :], gc[:, dch:dch + 1], a_, ALU.mult, ALU.add, [PT[by], t_modc, t_acc[dch]], [t_acc[dch]])
            for q4 in range(4):
                P.dma("sp", hT_v[:, q4 * 4:(q4 + 1) * 4, tsl], acc[:, q4 * 4:(q4 + 1) * 4, :],
                      reads=t_acc[q4 * 4:(q4 + 1) * 4], writes=t_hT[4 * th:4 * th + 4])
        P.barrier()
        AR.release(m)

    u2T_d = dscr("u2T", [D, S], BF16)
    u2T_v = u2T_d.ap().rearrange("(kc p) t -> p kc t", p=128)
    t_u2T = P.tok("u2T")

    t_mix = P.tok("mixT")

    def outproj_stage(l, w_out_ap, t_wout):
        m = AR.mark()
        stage_bufs()
        mx = [AR.alloc([128, KC, 512], BF16) for _ in range(2)]; t_mx = [P.tok() for _ in range(2)]
        wo = [AR.alloc([128, KC, 512], BF16) for _ in range(2)]; t_wo = [P.tok() for _ in range(2)]
        ht = [AR.alloc([128, 512], F32) for _ in range(3)]; t_ht = [P.tok() for _ in range(3)]
        gc = gate_col(l, 0)
        nw = 0
        nh = 0
        for tg in range(4):
            a, ta = mx[tg % 2], t_mx[tg % 2]
            P.dma("sp", a, mixT_v[:, :, tg * 512:(tg + 1) * 512], reads=[t_mix], writes=[ta])
            for db in range(4):
                wv, tw = wo[nw % 2], t_wo[nw % 2]
                nw += 1
                load_w(wv, w_out_ap, db * 512, 512, tw, t_wout)
                for j in range(4):
                    dch = db * 4 + j
                    h_, th_ = ht[nh % 3], t_ht[nh % 3]
                    nh += 1
                    P.dma("sp", h_, hT_v[:, dch, tg * 512:(tg + 1) * 512], reads=[t_hT[2 * tg], t_hT[2 * tg + 1]], writes=[th_])
                    b = bank()
                    for cc in range(KC):
                        MM(PB[b][:, :], wv[:, cc, j * 128:(j + 1) * 128], a[:, cc, :], cc == 0, cc == KC - 1, [tw, ta], [PT[b]])
                    STT("dve", h_, PB[b][:, :], gc[:, dch:dch + 1], h_, ALU.mult, ALU.add, [PT[b], t_modc, th_], [th_])
                    P.dma("sp", hT_v[:, dch, tg * 512:(tg + 1) * 512], h_, reads=[th_], writes=[t_hT[2 * tg], t_hT[2 * tg + 1]])
        P.barrier()
        AR.release(m)

    def final_stage():
        m = AR.mark()
        hg = [AR.alloc([128, KC, 128], F32) for _ in range(2)]; t_hg = [P.tok() for _ in range(2)]
        sq = [AR.alloc([128, 128], F32) for _ in range(3)]; t_sq = [P.tok() for _ in range(3)]
        rs = [AR.alloc([128, 128], F32) for _ in range(2)]; t_rs = [P.tok() for _ in range(2)]
        tmp = [AR.alloc([128, 128], F32) for _ in range(3)]; t_tmp = [P.tok() for _ in range(3)]
        ot = [AR.alloc([128, D], F32) for _ in range(2)]; t_ot = [P.tok() for _ in range(2)]
        n3 = 0
        for t in range(NT):
            h, th = hg[t % 2], t_hg[t % 2]
            o, to = ot[t % 2], t_ot[t % 2]
            P.dma("sp", h, hT_v[:, :, t * 128:(t + 1) * 128], reads=[t_hT[t // 2]], writes=[th])
            b = bank()
            for kc in range(KC):
                s_, ts_ = sq[n3 % 3], t_sq[n3 % 3]
                n3 += 1
                ACT(s_, h[:, kc, :], AF.Square, [th], [ts_])
                MM(PB[b][:, 0:128], ones_f, s_, kc == 0, kc == KC - 1, [t_ones, ts_], [PT[b]])
            r_, tr_ = rs[t % 2], t_rs[t % 2]
            TS("dve", r_, PB[b][:, 0:128], 1.0 / D, EPS, ALU.mult, ALU.add, [PT[b]], [tr_])
            RSQRT(r_, tr_)
            for kq in range(4):
                b2 = bank()
                for j in range(4):
                    kc = kq * 4 + j
                    t_, tt_ = tmp[n3 % 3], t_tmp[n3 % 3]
                    n3 += 1
                    STT("dve", t_, h[:, kc, :], finalg[:, kc:kc + 1], r_, ALU.mult, ALU.mult, [th, t_finalg, tr_], [tt_])
                    TR(PB[b2][:, j * 128:(j + 1) * 128], t_, ident_f, [tt_, t_identf], [PT[b2]])
                CP("act", o[:, kq * 512:(kq + 1) * 512], PB[b2][:, :], [PT[b2]], [to])
            P.dma("sp", out_d.ap()[t * 128:(t + 1) * 128, :], o, reads=[to], writes=[t_out])
        P.barrier()
        AR.release(m)

    t_out = P.tok("out")

    NL = L if "l1" in stages else 1
    gw = {}

    def issue_gathers(l):
        g_ = {}
        g_["w_in"] = gather("w_in%d" % l, w_in_d.ap()[l], [D, INW])
        g_["w_out"] = gather("w_out%d" % l, w_out_d.ap()[l], [D, D])
        if "moe" in stages:
            g_["w1"] = gather("moe_w1_%d" % l, moe_w1_d.ap()[l], [NEXP, D, DEXP])
            g_["w3"] = gather("moe_w3_%d" % l, moe_w3_d.ap()[l], [NEXP, D, DEXP])
            g_["w2"] = gather("moe_w2_%d" % l, moe_w2_d.ap()[l], [NEXP, DEXP, D])
        gw[l] = g_

    issue_gathers(0)
    for l in range(NL):
        w_in_ap, t_win = gw[l]["w_in"]
        w_out_ap, t_wout = gw[l]["w_out"]
        m_l = AR.mark()
        uT = AR.alloc([128, KC, S], BF16)
        norm_stage(l, 0)
        if "dbg_u" in dbg and l == 0:
            dbg_out["dbg_u"] = nc.dram_tensor("dbg_u", [128, KC, S], BF16, kind="ExternalOutput")
            P.dma("sp", dbg_out["dbg_u"].ap(), uT, reads=t_uT)
        if "gla" in stages:
            gla_stage(l, w_in_ap, t_win)
        if "lru" in stages:
            lru_stage(l, w_in_ap, t_win)
        if "diff" in stages:
            diff_stage(l, w_in_ap, t_win)
        P.barrier()
        AR.release(m_l)
        if "outproj" in stages:
            outproj_stage(l, w_out_ap, t_wout)
        if "moe" in stages:
            m_w = AR.mark()
            wcT = AR.alloc([64, S], F32); t_wcT = P.tok()
            m_u = AR.mark()
            uT = AR.alloc([128, KC, S], BF16)
            router = make_router(l, wcT, t_wcT)
            norm_stage(l, 1, router)
            P.dma("sp", u2T_v, uT, reads=t_uT, writes=[t_u2T])
            if "dbg_wc" in dbg and l == 0:
                dbg_out["dbg_wc"] = nc.dram_tensor("dbg_wc", [64, S], F32, kind="ExternalOutput")
                P.dma("sp", dbg_out["dbg_wc"].ap(), wcT, reads=[t_wcT])
            P.barrier()
            AR.release(m_u)
            moe_stage(l, gw[l]["w1"][0], gw[l]["w3"][0], gw[l]["w2"][0], gw[l]["w1"][1], gw[l]["w3"][1], gw[l]["w2"][1], wcT, t_wcT)
            AR.release(m_w)
        if l + 1 < NL:
            issue_gathers(l + 1)
    if "dbg_mix" in dbg:
        dbg_out["dbg_mix"] = nc.dram_tensor("dbg_mix", [D, S], BF16, kind="ExternalOutput")
        P.dma("sp", dbg_out["dbg_mix"].ap(), mixT_d.ap(), reads=[t_mix])
    if "dbg_h" in dbg:
        dbg_out["dbg_h"] = nc.dram_tensor("dbg_h", [D, S], F32, kind="ExternalOutput")
        P.dma("sp", dbg_out["dbg_h"].ap(), hT_d.ap(), reads=t_hT)
    if "final" in stages:
        final_stage()
    P.emit()
    st.close()
    print("arena peak", AR.peak, "instr", {k: v.n for k, v in P.eng.items()})
    return nc


def _col(v):
    v = np.asarray(v, np.float32)
    lead = v.shape[:-1]
    n = v.shape[-1] // 128
    v = v.reshape(*lead, n, 128)
    return np.ascontiguousarray(np.moveaxis(v, -1, 0))


def _t5_bucket_np(rel):
    nb = 16
    ret = (rel > 0).astype(np.int32) * nb
    n = np.abs(rel)
    max_exact = nb // 2
    nf = np.maximum(n, 1).astype(np.float32)
    large = max_exact + (np.log(nf / max_exact) / math.log(128 / max_exact) * (nb - max_exact)).astype(np.int32)
    large = np.minimum(large, nb - 1)
    return ret + np.where(n < max_exact, n, large)


def const_inputs(stages):
    c = {}
    c["ident_f"] = np.eye(128, dtype=np.float32)
    c["ones_f"] = np.ones((128, 128), np.float32)
    if "gla" in stages:
        tp = np.arange(64)
        triC = (tp[:, None] <= tp[None, :]).astype(np.float32) / 16.0
        triR = (tp[:, None] > tp[None, :]).astype(np.float32) / 16.0
        full = np.ones((64, 64), np.float32) / 16.0
        c["gla_tri"] = np.stack([triC, triR, full]).astype(np.float32)
        mf = (tp[:, None] <= tp[None, :]).astype(np.float32)
        mb = (tp[:, None] > tp[None, :]).astype(np.float32)
        c["gla_mask"] = np.stack([np.tile(mf, (1, 3)), np.tile(mb, (1, 3))]).astype(np.float32)
    if "diff" in stages:
        k = np.arange(128)[:, None]
        jj = np.arange(256)[None, :]
        bkt = _t5_bucket_np((k - jj).astype(np.int32))
        oh = (bkt[:, None, :] == np.arange(32)[None, :, None]).astype(np.float32)
        c["t5_oh"] = np.ascontiguousarray(oh)
        mask = np.zeros((128, 256), np.float32)
        mask[64:, :64] = -200.0
        c["t5_mask"] = mask
    return c


def core_inputs(inp, b, R, stages):
    r = b % R
    JC = 96 // R
    NCOL = 12288 // R
    m = {}
    m["x"] = np.ascontiguousarray(inp["x"][b])
    m["c_col"] = np.ascontiguousarray(_col(inp["c"]).transpose(0, 2, 1))
    oh = np.zeros((128, R * L * JC, 8), np.float32)
    oh[:, :, b] = 1.0
    m["onehot_rep"] = oh
    m["ada_w_sh"] = np.ascontiguousarray(inp["ada_w"][:, :, r * NCOL:(r + 1) * NCOL])
    m["ada_b_col"] = _col(inp["ada_b"])
    m["norm_g_col"] = np.ascontiguousarray(np.stack([_col(inp["norm1_g"]), _col(inp["norm2_g"])], axis=2))
    m["final_g_col"] = _col(inp["final_g"])
    rows = D // R
    m["w_in_sh"] = np.ascontiguousarray(inp["w_in"][:, r * rows:(r + 1) * rows, :])
    m["w_out_sh"] = np.ascontiguousarray(inp["w_out"][:, r * rows:(r + 1) * rows, :])
    if "lru" in stages:
        cols = [inp["lru_conv_w"][:, i, :] for i in range(4)] + [inp["lru_conv_b"], inp["lru_ba"], inp["lru_bx"], inp["lru_lambda"]]
        m["lru_col"] = np.ascontiguousarray(np.stack([_col(v) for v in cols], axis=-1))
        wbd = np.zeros((L, 4, 2, 128, 128), np.float32)
        for wi, nm in enumerate(("lru_wa", "lru_wx")):
            for cc in range(4):
                for hb in range(2):
                    wbd[:, cc, wi, hb * 64:(hb + 1) * 64, hb * 64:(hb + 1) * 64] = inp[nm][:, 2 * cc + hb]
        m["lru_wbd"] = wbd
    if "gla" in stages:
        m["gla_wa2"] = np.ascontiguousarray(np.concatenate([inp["gla_w_a2"], inp["gla_b_a"][:, None, :]], axis=1))
        m["gla_ng"] = np.ascontiguousarray(inp["gla_norm_g"])
    if "diff" in stages:
        m["diff_l"] = np.ascontiguousarray(np.concatenate([inp["diff_lq1"], inp["diff_lk1"], inp["diff_lq2"], inp["diff_lk2"]], axis=1))
        m["diff_subln"] = np.ascontiguousarray(inp["diff_subln_g"])
        m["rel_bias"] = np.ascontiguousarray(inp["rel_bias"].reshape(1, 192))
    if "moe" in stages:
        m["router_w"] = np.ascontiguousarray(np.concatenate([inp["router_g_w"], inp["router_e_w"]], axis=2))
        m["router_b"] = np.ascontiguousarray(np.concatenate([inp["router_g_b"], inp["router_e_b"]], axis=1))
        ne = NEXP // R
        for nm in ("moe_w1", "moe_w3", "moe_w2"):
            m[nm + "_sh"] = np.ascontiguousarray(inp[nm][:, r * ne:(r + 1) * ne])
    m.update(const_inputs(stages))
    return m


ALL_STAGES = {"gla", "lru", "diff", "outproj", "moe", "final", "l1"}


def kernel(**inputs):
    R = 8
    inp = {k: np.asarray(v) for k, v in inputs.items()}
    nc = build(R, ALL_STAGES)
    in_maps = [core_inputs(inp, b, R, ALL_STAGES) for b in range(R)]
    res = run_bass_kernel_spmd(nc, in_maps, core_ids=list(range(R)))
    out = np.stack([np.asarray(res.results[b]["out"]) for b in range(R)], axis=0)
    return np.ascontiguousarray(out.astype(np.float32))
```

```python
import math
import numpy as np
from contextlib import ExitStack
import ml_dtypes
import concourse.bass as bass
import concourse.mybir as mybir
from concourse.bass_utils import run_bass_kernel_spmd

F32 = mybir.dt.float32
BF16 = mybir.dt.bfloat16
U32 = mybir.dt.uint32
U8 = mybir.dt.uint8
AF = mybir.ActivationFunctionType
ALU = mybir.AluOpType
AX = mybir.AxisListType

D = 2048
S = 2048
NT = 16
KC = 16
L = 2
EPS = 1e-6
INW = 5648
OFF_GQ, OFF_GK, OFF_GV, OFF_GOG, OFF_GA, OFF_LY, OFF_LX, OFF_DQ, OFF_DK, OFF_DV = (
    0, 384, 768, 1536, 2304, 2320, 2832, 3344, 4112, 4880)
NEXP = 64
DEXP = 512


class Tok:
    __slots__ = ("name", "w", "r", "ex")

    def __init__(self, name="", ex=False):
        self.name = name
        self.w = None
        self.r = {}
        self.ex = ex


class _Eng:
    def __init__(self, name, key):
        self.name = name
        self.key = key
        self.n = 0
        self.stream = []
        self.known = {}


class Prog:
    NDS = 32

    def __init__(self, nc, stack):
        self.nc = nc
        self.stack = stack
        self.sems = {}
        self.eng = {}
        for name in ("pe", "act", "dve", "pool", "sp"):
            key = "E_" + name
            self.sems[key] = stack.enter_context(nc.semaphore("s_" + name))
            self.eng[name] = _Eng(name, key)
        self.dkeys = []
        self.dcount = []
        for i in range(self.NDS):
            key = "D_%d" % i
            self.sems[key] = stack.enter_context(nc.semaphore("d_%d" % i))
            self.dkeys.append(key)
            self.dcount.append(0)
        self.drr = 0
        self.ncc = 0
        self.cc_limit = 10 ** 9
        self._tid = 0

    def tok(self, name="", ex=False):
        self._tid += 1
        return Tok(name or "t%d" % self._tid, ex)

    def _waits(self, e, reads, writes, is_dma):
        need = {}

        def add(ev):
            if ev is None:
                return
            k, v = ev
            if need.get(k, 0) < v:
                need[k] = v

        for t in reads:
            add(t.w)
            if t.ex:
                for k, v in t.r.items():
                    if k != e.key:
                        add((k, v))
        for t in writes:
            if t.w is not None and (is_dma or t.w[0] != e.key):
                add(t.w)
            for k, v in t.r.items():
                if is_dma or k != e.key:
                    add((k, v))
        for k, v in need.items():
            if e.known.get(k, 0) < v:
                e.known[k] = v
                e.stream.append(("wait", k, v))

    def _mark(self, ev, reads, writes):
        k, v = ev
        for t in reads:
            if t.r.get(k, 0) < v:
                t.r[k] = v
        for t in writes:
            t.w = ev
            t.r = {}

    def op(self, en, fn, reads=(), writes=()):
        e = self.eng[en]
        self._waits(e, reads, writes, False)
        e.n += 1
        e.stream.append(("ins", fn, e.key, 1))
        self._mark((e.key, e.n), reads, writes)

    def dma(self, qn, out, in_, reads=(), writes=(), **kw):
        self.custom(qn, (lambda eng: eng.dma_start(out=out, in_=in_, **kw)), reads, writes)

    def custom(self, qn, fn, reads=(), writes=()):
        e = self.eng[qn]
        i = self.drr
        self.drr = (i + 1) % self.NDS
        k = self.dkeys[i]
        if self.dcount[i] > 0:
            v = self.dcount[i] * 16
            if e.known.get(k, 0) < v:
                e.known[k] = v
                e.stream.append(("wait", k, v))
        self._waits(e, reads, writes, True)
        self.dcount[i] += 1
        e.stream.append(("ins", fn, k, 16))
        self._mark((k, self.dcount[i] * 16), reads, writes)

    def collective(self, fn, reads=(), writes=()):
        e = self.eng["pool"]
        key = "C_%d" % self.ncc
        self.ncc += 1
        self.sems[key] = self.stack.enter_context(self.nc.semaphore("c_%d" % self.ncc))
        self._waits(e, reads, writes, True)
        e.stream.append(("ins", fn, key, 1))
        self._mark((key, 1), reads, writes)

    def barrier(self):
        evs = []
        for o in self.eng.values():
            if o.n > 0:
                evs.append((o.key, o.n))
        for i, k in enumerate(self.dkeys):
            if self.dcount[i] > 0:
                evs.append((k, self.dcount[i] * 16))
        for i in range(min(self.ncc, self.cc_limit)):
            evs.append(("C_%d" % i, 1))
        for e in self.eng.values():
            for k, v in evs:
                if k == e.key:
                    continue
                if e.known.get(k, 0) < v:
                    e.known[k] = v
                    e.stream.append(("wait", k, v))

    def emit(self):
        nc = self.nc
        self.cc_limit = 10 ** 9
        self.barrier()

        def replay(e, eng):
            for it in e.stream:
                if it[0] == "wait":
                    eng.wait_ge(self.sems[it[1]], it[2])
                else:
                    ins = it[1](eng)
                    ins.then_inc(self.sems[it[2]], it[3])

        with nc.Block() as block:
            @block.tensor
            def _(eng):
                replay(self.eng["pe"], eng)

            @block.scalar
            def _(eng):
                replay(self.eng["act"], eng)

            @block.vector
            def _(eng):
                replay(self.eng["dve"], eng)

            @block.gpsimd
            def _(eng):
                replay(self.eng["pool"], eng)

            @block.sync
            def _(eng):
                replay(self.eng["sp"], eng)


_DTSZ = {F32: 4, BF16: 2, U32: 4, U8: 1}


class Arena:
    def __init__(self, nc, nbytes):
        self.t = nc.alloc_sbuf_tensor("arena", [128, nbytes], U8)
        self.cap = nbytes
        self.off = 0
        self.peak = 0

    def alloc(self, shape, dt):
        sz = _DTSZ[dt]
        n = int(np.prod(shape[1:])) * sz
        n_al = (n + 31) // 32 * 32
        assert self.off + n_al <= self.cap, ("arena overflow", self.off, n_al, self.cap)
        ap = self.t[0:shape[0], self.off:self.off + n].bitcast(dt)
        if len(shape) == 3:
            ap = ap.rearrange("p (a b) -> p a b", a=shape[1])
        elif len(shape) == 4:
            ap = ap.rearrange("p (a b c) -> p a b c", a=shape[1], b=shape[2])
        self.off += n_al
        self.peak = max(self.peak, self.off)
        return ap

    def mark(self):
        return self.off

    def release(self, m):
        self.off = m


GDBG = {"stop": 99, "nhp": 3, "nt": 16}


def build(R, stages, dbg=()):
    nc = bass.Bass("TRN2", target_bir_lowering=False)
    st = ExitStack()
    P = Prog(nc, st)
    AR = Arena(nc, 206 * 1024)
    JC = 96 // R

    def din(name, shape, dt=F32):
        return nc.dram_tensor(name, list(shape), dt, kind="ExternalInput")

    def dscr(name, shape, dt=F32):
        return nc.dram_tensor(name, list(shape), dt)

    x_d = din("x", [S, D])
    c_col_d = din("c_col", [128, KC, 8])
    onehot_d = din("onehot_rep", [128, R * L * JC, 8])
    ada_w_d = din("ada_w_sh", [L, D, 12288 // R])
    ada_b_d = din("ada_b_col", [128, L, 96])
    normg_d = din("norm_g_col", [128, L, 2, KC])
    finalg_d = din("final_g_col", [128, KC])
    ident_d = din("ident_f", [128, 128])
    ones_d = din("ones_f", [128, 128])
    w_in_d = din("w_in_sh", [L, D // R, INW])
    w_out_d = din("w_out_sh", [L, D // R, D])
    if "lru" in stages:
        lru_col_d = din("lru_col", [128, L, 4, 8])
        lru_wbd_d = din("lru_wbd", [L, 4, 2, 128, 128])
    if "gla" in stages:
        gla_wa2_d = din("gla_wa2", [L, 17, 384])
        gla_ng_d = din("gla_ng", [L, 768])
        tri_d = din("gla_tri", [3, 64, 64])
        gmask_d = din("gla_mask", [2, 64, 192])
    if "diff" in stages:
        diff_l_d = din("diff_l", [L, 256])
        subln_d = din("diff_subln", [L, 128])
        relb_d = din("rel_bias", [1, 192])
        t5oh_d = din("t5_oh", [128, 32, 256])
        t5mask_d = din("t5_mask", [128, 256])
    if "moe" in stages:
        router_w_d = din("router_w", [L, D, 72])
        router_b_d = din("router_b", [L, 72])
        moe_w1_d = din("moe_w1_sh", [L, NEXP // R, D, DEXP])
        moe_w3_d = din("moe_w3_sh", [L, NEXP // R, D, DEXP])
        moe_w2_d = din("moe_w2_sh", [L, NEXP // R, DEXP, D])
    out_d = nc.dram_tensor("out", [S, D], F32, kind="ExternalOutput")
    dbg_out = {}

    hT_d = dscr("hT", [D, S])
    mixT_d = dscr("mixT", [D, S], BF16)
    hT_v = hT_d.ap().rearrange("(kc p) t -> p kc t", p=128)
    mixT_v = mixT_d.ap().rearrange("(kc p) t -> p kc t", p=128)

    NFB = 6
    PB = [nc.alloc_psum_tensor("pb%d" % i, [128, 512], F32) for i in range(NFB)]
    PT = [P.tok("pb%d" % i, ex=True) for i in range(NFB)]
    PBT = [nc.alloc_psum_tensor("pbt%d" % i, [128, 1024], BF16) for i in range(2)]
    PTT = [P.tok("pbt0", ex=True), P.tok("pbt1", ex=True)]
    bk = [0]
    bkt = [0]

    nrot = [NFB]

    def bank():
        i = bk[0] % nrot[0]
        bk[0] = (i + 1) % nrot[0]
        return i

    def tbank():
        i = bkt[0]
        bkt[0] = (i + 1) % 2
        return PBT[i], PTT[i]

    def MM(out, lhsT, rhs, start, stop, reads, writes):
        P.op("pe", lambda e: e.matmul(out, lhsT, rhs, start=start, stop=stop), reads, writes)

    def TR(out, in_, ident, reads, writes):
        P.op("pe", lambda e: e.transpose(out, in_, ident), reads, writes)

    def ACT(out, in_, func, reads, writes, **kw):
        P.op("act", lambda e: e.activation(out=out, in_=in_, func=func, **kw), reads, writes)

    def TT(en, out, in0, in1, op, reads, writes):
        P.op(en, lambda e: e.tensor_tensor(out=out, in0=in0, in1=in1, op=op), reads, writes)

    def TS(en, out, in0, s1, s2, op0, op1, reads, writes):
        if op1 is None:
            P.op(en, lambda e: e.tensor_scalar(out=out, in0=in0, scalar1=s1, scalar2=None, op0=op0), reads, writes)
        else:
            P.op(en, lambda e: e.tensor_scalar(out=out, in0=in0, scalar1=s1, scalar2=s2, op0=op0, op1=op1), reads, writes)

    def STT(en, out, in0, scalar, in1, op0, op1, reads, writes):
        P.op(en, lambda e: e.scalar_tensor_tensor(out=out, in0=in0, scalar=scalar, in1=in1, op0=op0, op1=op1), reads, writes)

    def CP(en, out, in_, reads, writes):
        if en == "act":
            P.op("act", lambda e: e.copy(out, in_), reads, writes)
        else:
            P.op(en, lambda e: e.tensor_copy(out, in_), reads, writes)

    def MEMSET(en, ap, val, writes):
        P.op(en, lambda e: e.memset(ap, val), (), writes)

    def RSQRT(ap, tok_):
        ACT(ap, ap, AF.Sqrt, [tok_], [tok_])
        P.op("dve", lambda e: e.reciprocal(out=ap, in_=ap), [tok_], [tok_])

    def RSUM(en, out, in_, reads, writes):
        P.op(en, lambda e: e.reduce_sum(out=out, in_=in_, axis=AX.X), reads, writes)

    def gather(name, src_ap, shape, dt=F32):
        t = P.tok(name)
        if R == 1:
            return src_ap, t
        n0 = shape[0] // R
        if len(shape) == 3:
            rows, cols = n0 * shape[1], shape[2]
        else:
            rows, cols = n0, shape[1]
        bounce = dscr(name + "_b", [rows, cols], dt)
        full = dscr(name + "_f", [R * rows, cols], dt)
        tbs = []
        if len(shape) == 3:
            for i in range(n0):
                tb = P.tok()
                P.dma("sp", bounce.ap()[i * shape[1]:(i + 1) * shape[1], :], src_ap[i], writes=[tb])
                tbs.append(tb)
            full_ap = full.ap().rearrange("(e k) c -> e k c", k=shape[1])
        else:
            tb = P.tok()
            P.dma("sp", bounce.ap(), src_ap, writes=[tb])
            tbs.append(tb)
            full_ap = full.ap()
        P.collective(lambda e: e.collective_compute("AllGather", ALU.bypass, replica_groups=[list(range(R))],
                                                    ins=[bounce.ap()], outs=[full.ap()]),
                     reads=tbs, writes=[t])
        return full_ap, t

    ident_f = AR.alloc([128, 128], F32); t_identf = P.tok()
    ident_b = AR.alloc([128, 128], BF16); t_identb = P.tok()
    ones_f = AR.alloc([128, 128], F32); t_ones = P.tok()
    modc = AR.alloc([128, L, 96], F32); t_modc = P.tok()
    geff = AR.alloc([128, L, 2, KC], F32); t_geff = P.tok()
    normg = AR.alloc([128, L, 2, KC], F32); t_normg = P.tok()
    finalg = AR.alloc([128, KC], F32); t_finalg = P.tok()
    uT = None
    t_uT = [P.tok("uT%d" % g) for g in range(8)]

    P.dma("sp", ident_f, ident_d.ap(), writes=[t_identf])
    P.dma("sp", ones_f, ones_d.ap(), writes=[t_ones])
    P.dma("sp", normg, normg_d.ap(), writes=[t_normg])
    P.dma("sp", finalg, finalg_d.ap(), writes=[t_finalg])
    CP("dve", ident_b, ident_f, [t_identf], [t_identb])

    m0 = AR.mark()
    c_col = AR.alloc([128, KC, 8], F32); t_c = P.tok()
    scT = AR.alloc([128, KC, 8], BF16); t_sc = P.tok()
    P.dma("sp", c_col, c_col_d.ap(), writes=[t_c])
    ACT(scT, c_col, AF.Silu, [t_c], [t_sc])
    modpart = AR.alloc([128, L * JC * 8], F32); t_mp = P.tok()
    NCOL = 12288 // R
    wblk = [AR.alloc([128, KC, 512], BF16) for _ in range(2)]
    t_wblk = [P.tok() for _ in range(2)]
    nb = 0
    for l in range(L):
        adaw_v = ada_w_d.ap()[l].rearrange("(kc p) c -> p kc c", p=128)
        for jb in range(NCOL // 512):
            wb, tw = wblk[nb % 2], t_wblk[nb % 2]
            nb += 1
            P.dma("pool", wb, adaw_v[:, :, jb * 512:(jb + 1) * 512], writes=[tw])
            b = bank()
            for j in range(4):
                for kc in range(KC):
                    MM(PB[b][:, j * 8:(j + 1) * 8], wb[:, kc, j * 128:(j + 1) * 128], scT[:, kc, :],
                       kc == 0, kc == KC - 1, [tw, t_sc], [PT[b]])
            jc0 = jb * 4
            c0 = (l * JC + jc0) * 8
            CP("dve", modpart[:, c0:c0 + 32], PB[b][:, 0:32], [PT[b]], [t_mp])
    if R > 1:
        ada_in = dscr("ada_in", [128, L * JC * 8])
        ada_all = dscr("ada_all", [R * 128, L * JC * 8])
        t_ai, t_aa = P.tok(), P.tok()
        P.dma("sp", ada_in.ap(), modpart, reads=[t_mp], writes=[t_ai])
        P.collective(lambda e: e.collective_compute("AllGather", ALU.bypass, replica_groups=[list(range(R))],
                                                    ins=[ada_in.ap()], outs=[ada_all.ap()]),
                     reads=[t_ai], writes=[t_aa])
        modall = AR.alloc([128, R, L * JC * 8], F32); t_ma = P.tok()
        P.dma("sp", modall, ada_all.ap().rearrange("(r p) f -> p r f", p=128), reads=[t_aa], writes=[t_ma])
        modall2 = modall.rearrange("p r f -> p (r f)")
    else:
        modall2, t_ma = modpart, t_mp
    onehot = AR.alloc([128, R * L * JC, 8], F32); t_oh = P.tok()
    P.dma("sp", onehot, onehot_d.ap(), writes=[t_oh])
    modsel3 = AR.alloc([128, R * L * JC, 8], F32); t_ms3 = P.tok()
    TT("dve", modsel3, modall2.rearrange("p (a b) -> p a b", b=8), onehot, ALU.mult, [t_ma, t_oh], [t_ms3])
    modsel = AR.alloc([128, R, L, JC], F32); t_ms = P.tok()
    RSUM("dve", modsel.rearrange("p r l j -> p (r l j)"), modsel3, [t_ms3], [t_ms])
    adab = AR.alloc([128, L, 96], F32); t_adab = P.tok()
    P.dma("sp", adab, ada_b_d.ap(), writes=[t_adab])
    for l in range(L):
        TT("dve", modc[:, l, :].rearrange("p (r j) -> p r j", r=R), modsel[:, :, l, :],
           adab[:, l, :].rearrange("p (r j) -> p r j", r=R), ALU.add, [t_ms, t_adab], [t_modc])
    tmp16 = AR.alloc([128, KC], F32); t_tmp16 = P.tok()
    for l in range(L):
        for w in range(2):
            sc = modc[:, l, 16 + 48 * w:32 + 48 * w]
            TS("dve", tmp16, sc, 1.0, None, ALU.add, None, [t_modc], [t_tmp16])
            TT("dve", geff[:, l, w, :], tmp16, normg[:, l, w, :], ALU.mult, [t_tmp16, t_normg], [t_geff])
    P.barrier()
    AR.release(m0)

    def sh_col(l, w):
        return modc[:, l, 48 * w:48 * w + 16]

    def gate_col(l, w):
        return modc[:, l, 32 + 48 * w:48 + 48 * w]

    m0 = AR.mark()
    xt = [AR.alloc([128, D], F32) for _ in range(2)]; t_xt = [P.tok() for _ in range(2)]
    xT = [AR.alloc([128, KC, 128], F32) for _ in range(2)]; t_xT = [P.tok() for _ in range(2)]
    t_hT = [P.tok("hT%d" % g) for g in range(8)]
    for t in range(NT):
        a, ta = xt[t % 2], t_xt[t % 2]
        o, to = xT[t % 2], t_xT[t % 2]
        P.dma("sp", a, x_d.ap()[t * 128:(t + 1) * 128, :], writes=[ta])
        for kq in range(4):
            b = bank()
            for j in range(4):
                kc = kq * 4 + j
                TR(PB[b][:, j * 128:(j + 1) * 128], a[:, kc * 128:(kc + 1) * 128], ident_f, [ta, t_identf], [PT[b]])
            CP("act" if kq % 2 else "dve", o[:, kq * 4:(kq + 1) * 4, :],
               PB[b][:, :].rearrange("p (a b) -> p a b", a=4), [PT[b]], [to])
        P.dma("sp", hT_v[:, :, t * 128:(t + 1) * 128], o, reads=[to], writes=[t_hT[t // 2]])
    P.barrier()
    AR.release(m0)

    def norm_stage(l, w, router=None):
        m = AR.mark()
        hg = [AR.alloc([128, KC, 256], F32) for _ in range(2)]; t_hg = [P.tok() for _ in range(2)]
        sq = [AR.alloc([128, 256], F32) for _ in range(3)]; t_sq = [P.tok() for _ in range(3)]
        rs = [AR.alloc([128, 256], F32) for _ in range(2)]; t_rs = [P.tok() for _ in range(2)]
        tmp = [AR.alloc([128, 256], F32) for _ in range(3)]; t_tmp = [P.tok() for _ in range(3)]
        uf = [AR.alloc([128, 256], F32) for _ in range(3)]; t_uf = [P.tok() for _ in range(3)]
        ge = geff[:, l, w, :]
        sh = sh_col(l, w)
        nsq = 0
        for g in range(8):
            h, th = hg[g % 2], t_hg[g % 2]
            P.dma("sp", h, hT_v[:, :, g * 256:(g + 1) * 256], reads=[t_hT[g]], writes=[th])
            b = bank()
            for kc in range(KC):
                s_, ts_ = sq[nsq % 3], t_sq[nsq % 3]
                nsq += 1
                ACT(s_, h[:, kc, :], AF.Square, [th], [ts_])
                MM(PB[b][:, 0:256], ones_f, s_, kc == 0, kc == KC - 1, [t_ones, ts_], [PT[b]])
            r_, tr_ = rs[g % 2], t_rs[g % 2]
            TS("dve", r_, PB[b][:, 0:256], 1.0 / D, EPS, ALU.mult, ALU.add, [PT[b]], [tr_])
            RSQRT(r_, tr_)
            if router is not None:
                rb = [bank(), bank()]
            for kc in range(KC):
                i3 = (g * KC + kc) % 3
                t_, tt_ = tmp[i3], t_tmp[i3]
                STT("dve", t_, h[:, kc, :], ge[:, kc:kc + 1], r_, ALU.mult, ALU.mult, [th, t_geff, tr_], [tt_])
                if router is None:
                    ACT(uT[:, kc, g * 256:(g + 1) * 256], t_, AF.Identity, [tt_, t_modc], [t_uT[g]],
                        bias=sh[:, kc:kc + 1], scale=1.0)
                else:
                    u_, tu_ = uf[i3], t_uf[i3]
                    ACT(u_, t_, AF.Identity, [tt_, t_modc], [tu_], bias=sh[:, kc:kc + 1], scale=1.0)
                    CP("pool", uT[:, kc, g * 256:(g + 1) * 256], u_, [tu_], [t_uT[g]])
                    for tt in range(2):
                        MM(PB[rb[tt]][:, 0:72], u_[:, tt * 128:(tt + 1) * 128], router["w"][:, kc, :],
                           kc == 0, kc == KC - 1, [tu_, router["tw"]], [PT[rb[tt]]])
            if router is not None:
                for tt in range(2):
                    router["post"](g * 2 + tt, rb[tt])
        P.barrier()
        AR.release(m)

    def load_w(dst, src_ap, c0, ncols, tw, tsrc):
        v = src_ap.rearrange("(kc p) c -> p kc c", p=128)
        P.dma("pool", dst, v[:, :, c0:c0 + ncols], reads=[tsrc], writes=[tw])

    def proj_fm(b, wt, tw, j0, ncol, tg):
        for kc in range(KC):
            MM(PB[b][0:ncol, :], wt[:, kc, j0:j0 + ncol], uT[:, kc, tg * 512:(tg + 1) * 512],
               kc == 0, kc == KC - 1, [tw, t_uT[2 * tg], t_uT[2 * tg + 1]], [PT[b]])

    def lru_stage(l, w_in_ap, t_win):
        m = AR.mark()
        lcol = AR.alloc([128, 4, 8], F32); t_lcol = P.tok()
        P.dma("sp", lcol, lru_col_d.ap()[:, l, :, :], writes=[t_lcol])
        cl = AR.alloc([128, 4, 2], F32); t_cl = P.tok()
        e1 = AR.alloc([128, 4], F32); t_e1 = P.tok()
        ACT(e1, lcol[:, :, 7], AF.Exp, [t_lcol], [t_e1], scale=-1.0)
        ACT(e1, e1, AF.Ln, [t_e1], [t_e1], bias=1.0, scale=1.0)
        TS("dve", cl[:, :, 0], e1, -8.0, None, ALU.mult, None, [t_e1], [t_cl])
        TS("dve", cl[:, :, 1], e1, -16.0, None, ALU.mult, None, [t_e1], [t_cl])
        wy = [AR.alloc([128, KC, 128], BF16) for _ in range(2)]; t_wy = [P.tok() for _ in range(2)]
        wx = [AR.alloc([128, KC, 128], BF16) for _ in range(2)]; t_wx = [P.tok() for _ in range(2)]
        wbd = [AR.alloc([128, 2, 128], F32) for _ in range(2)]; t_wbd = [P.tok() for _ in range(2)]
        xpad = AR.alloc([128, 3 + S], F32); t_xpad = P.tok()
        xc = AR.alloc([128, S], F32); t_xc = P.tok()
        r_ = AR.alloc([128, S], F32); t_r = P.tok()
        ig = AR.alloc([128, S], F32); t_ig = P.tok()
        a_ = AR.alloc([128, S], F32); t_a = P.tok()
        b_ = AR.alloc([128, S], F32); t_b = P.tok()
        yv = AR.alloc([128, S], F32); t_yv = P.tok()
        y2 = AR.alloc([128, S], F32); t_y2 = P.tok()
        ob = [AR.alloc([128, S], BF16) for _ in range(2)]; t_ob = [P.tok() for _ in range(2)]
        MEMSET("dve", xpad[:, 0:3], 0.0, [t_xpad])
        for cc in range(4):
            i2 = cc % 2
            load_w(wx[i2], w_in_ap, OFF_LX + cc * 128, 128, t_wx[i2], t_win)
            load_w(wy[i2], w_in_ap, OFF_LY + cc * 128, 128, t_wy[i2], t_win)
            P.dma("sp", wbd[i2], lru_wbd_d.ap()[l, cc].rearrange("w k m -> k w m"), writes=[t_wbd[i2]])
            for tg in range(4):
                b = bank()
                proj_fm(b, wx[i2], t_wx[i2], 0, 128, tg)
                CP("act", xpad[:, 3 + tg * 512:3 + (tg + 1) * 512], PB[b][:, :], [PT[b]], [t_xpad])
            TS("dve", xc, xpad[:, 0:S], lcol[:, cc, 0:1], lcol[:, cc, 4:5], ALU.mult, ALU.add, [t_xpad, t_lcol], [t_xc])
            for i in range(1, 4):
                STT("dve", xc, xpad[:, i:i + S], lcol[:, cc, i:i + 1], xc, ALU.mult, ALU.add, [t_xpad, t_lcol, t_xc], [t_xc])
            for tg in range(4):
                b = bank()
                MM(PB[b][:, :], wbd[i2][:, 0, :], xc[:, tg * 512:(tg + 1) * 512], True, True, [t_wbd[i2], t_xc], [PT[b]])
                ACT(r_[:, tg * 512:(tg + 1) * 512], PB[b][:, :], AF.Sigmoid, [PT[b], t_lcol], [t_r], bias=lcol[:, cc, 5:6], scale=1.0)
                b = bank()
                MM(PB[b][:, :], wbd[i2][:, 1, :], xc[:, tg * 512:(tg + 1) * 512], True, True, [t_wbd[i2], t_xc], [PT[b]])
                ACT(ig[:, tg * 512:(tg + 1) * 512], PB[b][:, :], AF.Sigmoid, [PT[b], t_lcol], [t_ig], bias=lcol[:, cc, 6:7], scale=1.0)
            ACT(a_, r_, AF.Exp, [t_r, t_cl], [t_a], scale=cl[:, cc, 0:1])
            ACT(b_, r_, AF.Exp, [t_r, t_cl], [t_b], scale=cl[:, cc, 1:2])
            TS("dve", b_, b_, -1.0, 1.0, ALU.mult, ALU.add, [t_b], [t_b])
            ACT(b_, b_, AF.Sqrt, [t_b], [t_b])
            TT("dve", ig, ig, xc, ALU.mult, [t_ig, t_xc], [t_ig])
            TT("dve", b_, b_, ig, ALU.mult, [t_b, t_ig], [t_b])
            P.op("dve", lambda e, a_=a_, b_=b_, r_=r_: e.tensor_tensor_scan(out=r_, data0=a_, data1=b_, initial=0.0,
                                                                         op0=ALU.mult, op1=ALU.add),
                 [t_a, t_b], [t_r])
            for tg in range(4):
                b = bank()
                proj_fm(b, wy[i2], t_wy[i2], 0, 128, tg)
                CP("act", yv[:, tg * 512:(tg + 1) * 512], PB[b][:, :], [PT[b]], [t_yv])
            ACT(y2, yv, AF.Square, [t_yv], [t_y2])
            TS("dve", y2, y2, 0.044715, 1.0, ALU.mult, ALU.add, [t_y2], [t_y2])
            TT("dve", y2, y2, yv, ALU.mult, [t_y2, t_yv], [t_y2])
            ACT(y2, y2, AF.Sigmoid, [t_y2], [t_y2], scale=1.5957691216057308)
            TT("dve", y2, y2, yv, ALU.mult, [t_y2, t_yv], [t_y2])
            TT("dve", ob[i2], r_, y2, ALU.mult, [t_r, t_y2], [t_ob[i2]])
            P.dma("sp", mixT_v[:, 6 + cc, :], ob[i2], reads=[t_ob[i2]], writes=[t_mix])
        P.barrier()
        AR.release(m)


    def gla_stage(l, w_in_ap, t_win):
        m = AR.mark()
        tri = AR.alloc([64, 3, 64], F32); t_tri = P.tok()
        P.dma("sp", tri, tri_d.ap()[:, 0:64, 0:64].rearrange("a k m -> k a m"), writes=[t_tri])
        gmask = AR.alloc([64, 2, 192], F32); t_gmask = P.tok()
        P.dma("sp", gmask, gmask_d.ap().rearrange("a k m -> k a m"), writes=[t_gmask])
        wa2 = AR.alloc([17, 384], F32); t_wa2 = P.tok()
        P.dma("sp", wa2, gla_wa2_d.ap()[l], writes=[t_wa2])
        gn = AR.alloc([64, 768], F32); t_gn = P.tok()
        P.dma("sp", gn, gla_ng_d.ap()[l:l + 1, :].partition_broadcast(64), writes=[t_gn])
        wga = AR.alloc([128, KC, 16], BF16); t_wga = P.tok()
        load_w(wga, w_in_ap, OFF_GA, 16, t_wga, t_win)
        gaT = AR.alloc([17, S], F32); t_gaT = P.tok()
        MEMSET("pool", gaT, 1.0, [t_gaT])
        for tg in range(4):
            b = bank()
            proj_fm(b, wga, t_wga, 0, 16, tg)
            CP("act", gaT[0:16, tg * 512:(tg + 1) * 512], PB[b][0:16, :], [PT[b]], [t_gaT])
        wq = AR.alloc([128, KC, 192], BF16); t_wq = P.tok()
        wk = AR.alloc([128, KC, 192], BF16); t_wk = P.tok()
        wv = AR.alloc([128, KC, 384], BF16); t_wv = P.tok()
        wog = AR.alloc([128, KC, 384], BF16); t_wog = P.tok()
        S_ = AR.alloc([64, 3, 128], F32); t_S = P.tok()
        Sb = AR.alloc([64, 3, 128], BF16); t_Sb = P.tok()
        mixg = AR.alloc([128, 3, S], BF16); t_mixg = P.tok()

        def buf2(shape, dt):
            return [AR.alloc(shape, dt) for _ in range(2)], [P.tok() for _ in range(2)]
        zA, t_zA = buf2([64, 192], F32)
        zB, t_zB = buf2([64, 192], F32)
        la, t_la = buf2([64, 192], F32)
        eG, t_eG = buf2([64, 192], F32)
        enG, t_enG = buf2([64, 192], F32)
        eR, t_eR = buf2([64, 192], F32)
        dec, t_dec = buf2([64, 3], F32)
        V, t_V = buf2([64, 384], BF16)
        SG, t_SG = buf2([64, 384], F32)
        KD, t_KD = buf2([64, 192], BF16)
        X4, t_X4 = buf2([64, 4, 192], BF16)
        XT, t_XT = buf2([64, 768], BF16)
        s1, t_s1 = buf2([64, 192], F32)
        s2, t_s2 = buf2([64, 192], F32)
        at, t_at = buf2([64, 192], BF16)
        sq, t_sq = buf2([64, 384], F32)
        ss, t_ss = buf2([64, 3], F32)
        y, t_y = buf2([64, 384], F32)
        yb, t_yb = buf2([64, 384], BF16)
        idb = ident_b[0:64, 0:64]
        for hg in range(GDBG["nhp"] if GDBG["nhp"] < 3 else 2):
            load_w(wq, w_in_ap, OFF_GQ + hg * 192, 192, t_wq, t_win)
            load_w(wk, w_in_ap, OFF_GK + hg * 192, 192, t_wk, t_win)
            load_w(wv, w_in_ap, OFF_GV + hg * 384, 384, t_wv, t_win)
            load_w(wog, w_in_ap, OFF_GOG + hg * 384, 384, t_wog, t_win)
            MEMSET("dve", S_, 0.0, [t_S])
            MEMSET("dve", Sb, 0.0, [t_Sb])
            for n in range(2 * GDBG["nt"]):
                j2 = n % 2
                tk = slice(n * 64, (n + 1) * 64)
                tu = [t_uT[n // 4]]
                b = bank()
                MM(PB[b][0:64, 0:192], gaT[0:17, tk], wa2[0:17, hg * 192:(hg + 1) * 192], True, True, [t_gaT, t_wa2], [PT[b]])
                ACT(zA[j2], PB[b][0:64, 0:192], AF.Relu, [PT[b]], [t_zA[j2]], scale=-1.0)
                ACT(zB[j2], PB[b][0:64, 0:192], AF.Abs, [PT[b]], [t_zB[j2]])
                ACT(zB[j2], zB[j2], AF.Exp, [t_zB[j2]], [t_zB[j2]], scale=-1.0)
                ACT(zB[j2], zB[j2], AF.Ln, [t_zB[j2]], [t_zB[j2]], bias=1.0, scale=1.0)
                STT("dve", la[j2], zB[j2], -1.0, zA[j2], ALU.mult, ALU.subtract, [t_zA[j2], t_zB[j2]], [t_la[j2]])
                bq = bank()
                for kc in range(KC):
                    MM(PB[bq][0:64, 0:192], uT[:, kc, tk], wq[:, kc, :], kc == 0, kc == KC - 1, tu + [t_wq], [PT[bq]])
                for kc in range(KC):
                    MM(PB[bq][0:64, 256:448], uT[:, kc, tk], wk[:, kc, :], kc == 0, kc == KC - 1, tu + [t_wk], [PT[bq]])
                bv = bank()
                for kc in range(KC):
                    MM(PB[bv][0:64, 0:384], uT[:, kc, tk], wv[:, kc, :], kc == 0, kc == KC - 1, tu + [t_wv], [PT[bv]])
                bo = bank()
                for kc in range(KC):
                    MM(PB[bo][0:64, 0:384], uT[:, kc, tk], wog[:, kc, :], kc == 0, kc == KC - 1, tu + [t_wog], [PT[bo]])
                bg = bank()
                MM(PB[bg][0:64, 0:192], tri[:, 0, :], la[j2], True, True, [t_tri, t_la[j2]], [PT[bg]])
                MM(PB[bg][0:64, 256:448], tri[:, 1, :], la[j2], True, True, [t_tri, t_la[j2]], [PT[bg]])
                for hh in range(3):
                    MM(PB[bg][0:64, 480 + hh:481 + hh], la[j2][:, hh * 64:(hh + 1) * 64], tri[:, 2, 0:1], True, True,
                       [t_tri, t_la[j2]], [PT[bg]])
                ACT(eG[j2], PB[bg][0:64, 0:192], AF.Exp, [PT[bg]], [t_eG[j2]])
                ACT(enG[j2], PB[bg][0:64, 0:192], AF.Exp, [PT[bg]], [t_enG[j2]], scale=-1.0)
                ACT(eR[j2], PB[bg][0:64, 256:448], AF.Exp, [PT[bg]], [t_eR[j2]])
                ACT(dec[j2], PB[bg][0:64, 480:483], AF.Exp, [PT[bg]], [t_dec[j2]])
                CP("act", V[j2], PB[bv][0:64, 0:384], [PT[bv]], [t_V[j2]])
                ACT(SG[j2], PB[bo][0:64, 0:384], AF.Silu, [PT[bo]], [t_SG[j2]])
                TT("dve", KD[j2], PB[bq][0:64, 256:448], eR[j2], ALU.mult, [PT[bq], t_eR[j2]], [t_KD[j2]])
                STT("dve", X4[j2][:, 0, :], PB[bq][0:64, 0:192], 0.125, eG[j2], ALU.mult, ALU.mult, [PT[bq], t_eG[j2]], [t_X4[j2]])
                TT("dve", X4[j2][:, 1, :], PB[bq][0:64, 256:448], enG[j2], ALU.mult, [PT[bq], t_enG[j2]], [t_X4[j2]])
                STT("dve", X4[j2][:, 2, :], PB[bq][0:64, 0:192], 0.125, enG[j2], ALU.mult, ALU.mult, [PT[bq], t_enG[j2]], [t_X4[j2]])
                TT("dve", X4[j2][:, 3, :], PB[bq][0:64, 256:448], eG[j2], ALU.mult, [PT[bq], t_eG[j2]], [t_X4[j2]])
                pbb, tpb = tbank()
                for w in range(4):
                    for hh in range(3):
                        c0 = (w * 3 + hh) * 64
                        TR(pbb[0:64, c0:c0 + 64], X4[j2][:, w, hh * 64:(hh + 1) * 64], idb, [t_X4[j2], t_identb], [tpb])
                CP("act", XT[j2], pbb[0:64, 0:768], [tpb], [t_XT[j2]])

                def xt(w, hh):
                    c0 = (w * 3 + hh) * 64
                    return XT[j2][:, c0:c0 + 64]
                bs = bank()
                for hh in range(3):
                    MM(PB[bs][0:64, hh * 64:(hh + 1) * 64], xt(1, hh), xt(0, hh), True, True, [t_XT[j2]], [PT[bs]])
                    MM(PB[bs][0:64, 256 + hh * 64:256 + (hh + 1) * 64], xt(3, hh), xt(2, hh), True, True, [t_XT[j2]], [PT[bs]])
                TT("dve", s1[j2], PB[bs][0:64, 0:192], gmask[:, 0, :], ALU.mult, [PT[bs], t_gmask], [t_s1[j2]])
                TT("dve", s2[j2], PB[bs][0:64, 256:448], gmask[:, 1, :], ALU.mult, [PT[bs], t_gmask], [t_s2[j2]])
                TT("dve", at[j2], s1[j2], s2[j2], ALU.add, [t_s1[j2], t_s2[j2]], [t_at[j2]])
                bo2 = bank()
                bkv = bank()
                for hh in range(3):
                    o_ap = PB[bo2][0:64, hh * 128:(hh + 1) * 128]
                    MM(o_ap, at[j2][:, hh * 64:(hh + 1) * 64], V[j2][:, hh * 128:(hh + 1) * 128], True, False, [t_at[j2], t_V[j2]], [PT[bo2]])
                    MM(o_ap, xt(0, hh), Sb[:, hh, :], False, True, [t_XT[j2], t_Sb], [PT[bo2]])
                for hh in range(3):
                    MM(PB[bkv][0:64, hh * 128:(hh + 1) * 128], KD[j2][:, hh * 64:(hh + 1) * 64], V[j2][:, hh * 128:(hh + 1) * 128], True, True,
                       [t_KD[j2], t_V[j2]], [PT[bkv]])
                for hh in range(3):
                    STT("dve", S_[:, hh, :], S_[:, hh, :], dec[j2][:, hh:hh + 1], PB[bkv][0:64, hh * 128:(hh + 1) * 128], ALU.mult, ALU.add,
                        [t_S, t_dec[j2], PT[bkv]], [t_S])
                CP("act", Sb, S_, [t_S], [t_Sb])
                ACT(sq[j2], PB[bo2][0:64, 0:384], AF.Square, [PT[bo2]], [t_sq[j2]])
                RSUM("dve", ss[j2], sq[j2].rearrange("p (a b) -> p a b", a=3), [t_sq[j2]], [t_ss[j2]])
                TS("dve", ss[j2], ss[j2], 1.0 / 128, EPS, ALU.mult, ALU.add, [t_ss[j2]], [t_ss[j2]])
                RSQRT(ss[j2], t_ss[j2])
                for hh in range(3):
                    TS("dve", y[j2][:, hh * 128:(hh + 1) * 128], PB[bo2][0:64, hh * 128:(hh + 1) * 128], ss[j2][:, hh:hh + 1], None,
                       ALU.mult, None, [PT[bo2], t_ss[j2]], [t_y[j2]])
                TT("dve", y[j2], y[j2], gn[:, hg * 384:(hg + 1) * 384], ALU.mult, [t_y[j2], t_gn], [t_y[j2]])
                TT("dve", yb[j2], y[j2], SG[j2], ALU.mult, [t_y[j2], t_SG[j2]], [t_yb[j2]])
                pbb, tpb = tbank()
                for hh in range(3):
                    TR(pbb[:, hh * 64:(hh + 1) * 64], yb[j2][:, hh * 128:(hh + 1) * 128], idb, [t_yb[j2], t_identb], [tpb])
                CP("act", mixg[:, :, tk], pbb[:, 0:192].rearrange("p (a b) -> p a b", a=3), [tpb], [t_mixg])
            P.dma("sp", mixT_v[:, 3 * hg:3 * hg + 3, :], mixg, reads=[t_mixg], writes=[t_mix])
        P.barrier()
        AR.release(m)

    def diff_stage(l, w_in_ap, t_win):
        m = AR.mark()
        DST = GDBG.get("dstop", 99)
        lam_init = 0.8 - 0.6 * math.exp(-0.3 * l)
        relb = AR.alloc([128, 192], F32); t_relb = P.tok()
        P.dma("sp", relb, relb_d.ap().partition_broadcast(128), writes=[t_relb])
        BT = AR.alloc([128, 6, 256], F32); t_BT = P.tok()
        m2 = AR.mark()
        oh = AR.alloc([128, 32, 256], F32); t_ohh = P.tok()
        P.dma("sp", oh, t5oh_d.ap(), writes=[t_ohh])
        tmask = AR.alloc([128, 256], F32); t_tmask = P.tok()
        P.dma("sp", tmask, t5mask_d.ap(), writes=[t_tmask])
        for h in range(6):
            TS("dve", BT[:, h, :], oh[:, 0, :], relb[:, h:h + 1], None, ALU.mult, None, [t_ohh, t_relb], [t_BT])
            for bq in range(1, 32):
                STT("dve", BT[:, h, :], oh[:, bq, :], relb[:, bq * 6 + h:bq * 6 + h + 1], BT[:, h, :], ALU.mult, ALU.add,
                    [t_ohh, t_relb, t_BT], [t_BT])
            TT("dve", BT[:, h, :], BT[:, h, :], tmask, ALU.add, [t_BT, t_tmask], [t_BT])
        P.barrier()
        AR.release(m2)
        dl = AR.alloc([128, 256], F32); t_dl = P.tok()
        P.dma("sp", dl, diff_l_d.ap()[l:l + 1, :].partition_broadcast(128), writes=[t_dl])
        pr = AR.alloc([128, 128], F32); t_pr = P.tok()
        TT("dve", pr[:, 0:64], dl[:, 0:64], dl[:, 64:128], ALU.mult, [t_dl], [t_pr])
        TT("dve", pr[:, 64:128], dl[:, 128:192], dl[:, 192:256], ALU.mult, [t_dl], [t_pr])
        e2 = AR.alloc([128, 2], F32); t_e2 = P.tok()
        RSUM("dve", e2, pr.rearrange("p (a b) -> p a b", a=2), [t_pr], [t_e2])
        ACT(e2, e2, AF.Exp, [t_e2], [t_e2])
        nlam = AR.alloc([128, 1], F32); t_nlam = P.tok()
        TT("dve", nlam, e2[:, 1:2], e2[:, 0:1], ALU.subtract, [t_e2], [t_nlam])
        TS("dve", nlam, nlam, -lam_init, None, ALU.add, None, [t_nlam], [t_nlam])
        sg = AR.alloc([128, 128], F32); t_sg = P.tok()
        P.dma("sp", sg, subln_d.ap()[l:l + 1, :].partition_broadcast(128), writes=[t_sg])
        TS("dve", sg, sg, 1.0 - lam_init, None, ALU.mult, None, [t_sg], [t_sg])

        if DST <= 1:
            dbg_out["dbg_bt"] = nc.dram_tensor("dbg_bt", [128, 6, 256], F32, kind="ExternalOutput")
            P.dma("sp", dbg_out["dbg_bt"].ap(), BT, reads=[t_BT])
            P.barrier()
            AR.release(m)
            return

        def buf2(shape, dt, n=2):
            return [AR.alloc(shape, dt) for _ in range(n)], [P.tok() for _ in range(n)]
        wq, t_wq = buf2([128, KC, 128], BF16)
        wk, t_wk = buf2([128, KC, 128], BF16)
        wv, t_wv = buf2([128, KC, 128], BF16)
        QT, t_QT = buf2([64, 2, S], BF16)
        KT, t_KT = buf2([64, 2, S], BF16)
        VE, t_VE = buf2([128, NT, 144], BF16)
        mixd, t_mixd = buf2([128, S], BF16)
        PTb, t_PTb = buf2([128, 512], BF16, 3)
        scb, t_scb = buf2([128, 128], F32, 3)
        rr, t_rr = buf2([128, 2], F32)
        dd, t_dd = buf2([128, 128], F32)
        sq, t_sq = buf2([128, 128], F32)
        ss, t_ss = buf2([128, 1], F32)
        yb, t_yb = buf2([128, 128], BF16)
        npt = 0
        nsc = 0
        nrot[0] = 4
        for h in range(GDBG.get("dnh", 6)):
            i2 = h % 2
            load_w(wq[i2], w_in_ap, OFF_DQ + h * 128, 128, t_wq[i2], t_win)
            load_w(wk[i2], w_in_ap, OFF_DK + h * 128, 128, t_wk[i2], t_win)
            load_w(wv[i2], w_in_ap, OFF_DV + h * 128, 128, t_wv[i2], t_win)
            MEMSET("dve", VE[i2].rearrange("p a b -> p (a b)"), 1.0, [t_VE[i2]])
            for tg in range(4):
                for mm_ in range(2):
                    b = bank()
                    proj_fm(b, wq[i2], t_wq[i2], mm_ * 64, 64, tg)
                    ACT(QT[i2][:, mm_, tg * 512:(tg + 1) * 512], PB[b][0:64, :], AF.Identity, [PT[b]], [t_QT[i2]], scale=0.125)
                    b = bank()
                    proj_fm(b, wk[i2], t_wk[i2], mm_ * 64, 64, tg)
                    CP("dve", KT[i2][:, mm_, tg * 512:(tg + 1) * 512], PB[b][0:64, :], [PT[b]], [t_KT[i2]])
            for t in range(NT):
                b = bank()
                for kc in range(KC):
                    MM(PB[b][:, 0:128], uT[:, kc, t * 128:(t + 1) * 128], wv[i2][:, kc, :], kc == 0, kc == KC - 1,
                       [t_uT[t // 2], t_wv[i2]], [PT[b]])
                CP("act", VE[i2][:, t, 0:128], PB[b][:, 0:128], [PT[b]], [t_VE[i2]])
            for qt in range(GDBG.get("dnq", NT) if DST > 2 else 0):
                j2 = qt % 2
                ql = slice(qt * 128, (qt + 1) * 128)
                bO = 4 + (qt % 2)
                nk = qt + 1
                for mm_ in range(2):
                    rw = slice(mm_ * 64, mm_ * 64 + 64)
                    for g0 in range(0, nk, 4):
                        kts = list(range(g0, min(g0 + 4, nk)))
                        bs = bank()
                        for i, kt in enumerate(kts):
                            MM(PB[bs][:, i * 128:(i + 1) * 128], KT[i2][:, mm_, kt * 128:(kt + 1) * 128], QT[i2][:, mm_, ql], True, True,
                               [t_KT[i2], t_QT[i2]], [PT[bs]])
                        pt, tpt = PTb[npt % 3], t_PTb[npt % 3]
                        npt += 1
                        nfar = len([kt for kt in kts if kt <= qt - 2])
                        if nfar:
                            ACT(pt[:, 0:nfar * 128], PB[bs][:, 0:nfar * 128], AF.Exp, [PT[bs], t_relb], [tpt],
                                bias=relb[:, 15 * 6 + h:15 * 6 + h + 1], scale=1.0)
                        for i, kt in enumerate(kts):
                            if kt >= qt - 1:
                                j0 = 0 if kt == qt else 128
                                sc_, tsc_ = scb[nsc % 3], t_scb[nsc % 3]
                                nsc += 1
                                TT("dve", sc_, PB[bs][:, i * 128:(i + 1) * 128], BT[:, h, j0:j0 + 128], ALU.add, [PT[bs], t_BT], [tsc_])
                                ACT(pt[:, i * 128:(i + 1) * 128], sc_, AF.Exp, [tsc_], [tpt])
                        for i, kt in enumerate(kts if DST > 3 else []):
                            MM(PB[bO][:, mm_ * 256:mm_ * 256 + 130], pt[:, i * 128:(i + 1) * 128], VE[i2][:, kt, 0:130], kt == 0, kt == qt,
                               [tpt, t_VE[i2]], [PT[bO]])
                if DST <= 4:
                    continue
                P.op("dve", lambda e, o=rr[j2][:, 0:1], i=PB[bO][:, 128:129]: e.reciprocal(out=o, in_=i), [PT[bO]], [t_rr[j2]])
                P.op("dve", lambda e, o=rr[j2][:, 1:2], i=PB[bO][:, 384:385]: e.reciprocal(out=o, in_=i), [PT[bO]], [t_rr[j2]])
                TT("dve", rr[j2][:, 1:2], rr[j2][:, 1:2], nlam, ALU.mult, [t_rr[j2], t_nlam], [t_rr[j2]])
                TS("dve", dd[j2], PB[bO][:, 0:128], rr[j2][:, 0:1], None, ALU.mult, None, [PT[bO], t_rr[j2]], [t_dd[j2]])
                STT("dve", dd[j2], PB[bO][:, 256:384], rr[j2][:, 1:2], dd[j2], ALU.mult, ALU.add, [PT[bO], t_rr[j2], t_dd[j2]], [t_dd[j2]])
                ACT(sq[j2], dd[j2], AF.Square, [t_dd[j2]], [t_sq[j2]])
                RSUM("dve", ss[j2], sq[j2], [t_sq[j2]], [t_ss[j2]])
                TS("dve", ss[j2], ss[j2], 1.0 / 128, EPS, ALU.mult, ALU.add, [t_ss[j2]], [t_ss[j2]])
                RSQRT(ss[j2], t_ss[j2])
                STT("dve", yb[j2], dd[j2], ss[j2][:, 0:1], sg, ALU.mult, ALU.mult, [t_dd[j2], t_ss[j2], t_sg], [t_yb[j2]])
                pbb, tpb = tbank()
                TR(pbb[:, 0:128], yb[j2], ident_b, [t_yb[j2], t_identb], [tpb])
                CP("act", mixd[i2][:, ql], pbb[:, 0:128], [tpb], [t_mixd[i2]])
            P.dma("sp", mixT_v[:, 10 + h, :], mixd[i2], reads=[t_mixd[i2]], writes=[t_mix])
        nrot[0] = NFB
        P.barrier()
        AR.release(m)

    def make_router(l, wcT, t_wcT):
        rw = AR.alloc([128, KC, 72], F32); t_rw = P.tok()
        P.dma("sp", rw, router_w_d.ap()[l].rearrange("(kc p) c -> p kc c", p=128), writes=[t_rw])
        rbias = AR.alloc([128, 72], F32); t_rb = P.tok()
        P.dma("sp", rbias, router_b_d.ap()[l:l + 1, :].partition_broadcast(128), writes=[t_rb])

        def buf2(shape, dt):
            return [AR.alloc(shape, dt) for _ in range(2)], [P.tok() for _ in range(2)]
        lg, t_lg = buf2([128, 72], F32)
        g8, t_g8 = buf2([128, 8], F32)
        sm, t_sm = buf2([128, 16], F32)
        ex, t_ex = buf2([128, 8], F32)
        ohg, t_ohg = buf2([128, 8], F32)
        lem, t_lem = buf2([128, 64], F32)
        e8, t_e8 = buf2([128, 8], F32)
        m1, t_m1 = buf2([128, 64], F32)
        m2, t_m2 = buf2([128, 64], F32)

        def post(t, rb):
            j = t % 2
            TT("dve", lg[j], PB[rb][:, 0:72], rbias, ALU.add, [PT[rb], t_rb], [t_lg[j]])
            P.op("dve", lambda e, o=g8[j], i=lg[j][:, 0:8]: e.max(out=o, in_=i), [t_lg[j]], [t_g8[j]])
            s_ = sm[j]
            ts_ = t_sm[j]
            TS("dve", s_[:, 0:1], g8[j][:, 0:1], -1.0, None, ALU.mult, None, [t_g8[j]], [ts_])
            ACT(ex[j], lg[j][:, 0:8], AF.Exp, [t_lg[j], ts_], [t_ex[j]], bias=s_[:, 0:1], scale=1.0)
            RSUM("dve", s_[:, 1:2], ex[j], [t_ex[j]], [ts_])
            P.op("dve", lambda e, o=s_[:, 2:3], i=s_[:, 1:2]: e.reciprocal(out=o, in_=i), [ts_], [ts_])
            TS("dve", ohg[j], lg[j][:, 0:8], g8[j][:, 0:1], None, ALU.is_equal, None, [t_lg[j], t_g8[j]], [t_ohg[j]])
            TS("dve", ohg[j], ohg[j], 1e9, -1e9, ALU.mult, ALU.add, [t_ohg[j]], [t_ohg[j]])
            for g in range(8):
                TS("dve", lem[j][:, g * 8:(g + 1) * 8], lg[j][:, 8 + g * 8:16 + g * 8], ohg[j][:, g:g + 1], None, ALU.add, None,
                   [t_lg[j], t_ohg[j]], [t_lem[j]])
            P.op("dve", lambda e, o=e8[j], i=lem[j]: e.max(out=o, in_=i), [t_lem[j]], [t_e8[j]])
            TT("dve", s_[:, 3:4], e8[j][:, 1:2], e8[j][:, 0:1], ALU.subtract, [t_e8[j]], [ts_])
            ACT(s_[:, 4:5], s_[:, 3:4], AF.Sigmoid, [ts_], [ts_], scale=-1.0)
            ACT(s_[:, 5:6], s_[:, 3:4], AF.Sigmoid, [ts_], [ts_])
            TT("dve", s_[:, 6:7], s_[:, 4:5], s_[:, 2:3], ALU.mult, [ts_], [ts_])
            TT("dve", s_[:, 7:8], s_[:, 5:6], s_[:, 2:3], ALU.mult, [ts_], [ts_])
            TS("dve", m1[j], lem[j], e8[j][:, 0:1], s_[:, 6:7], ALU.is_equal, ALU.mult, [t_lem[j], t_e8[j], ts_], [t_m1[j]])
            TS("dve", m2[j], lem[j], e8[j][:, 1:2], s_[:, 7:8], ALU.is_equal, ALU.mult, [t_lem[j], t_e8[j], ts_], [t_m2[j]])
            TT("dve", m1[j], m1[j], m2[j], ALU.add, [t_m1[j], t_m2[j]], [t_m1[j]])
            b = bank()
            TR(PB[b][0:64, 0:128], m1[j], ident_f, [t_m1[j], t_identf], [PT[b]])
            CP("act", wcT[:, t * 128:(t + 1) * 128], PB[b][0:64, 0:128], [PT[b]], [t_wcT])
        return {"w": rw, "tw": t_rw, "post": post}

    def moe_stage(l, w1_ap, w3_ap, w2_ap, t_w1, t_w3, t_w2, wcT, t_wcT):
        m = AR.mark()
        gc = gate_col(l, 1)
        u2h = AR.alloc([128, KC, 1024], BF16); t_u2h = P.tok()
        acc = AR.alloc([128, KC, 1024], F32); t_acc = [P.tok() for _ in range(KC)]

        def bufn(shape, dt, n=2):
            return [AR.alloc(shape, dt) for _ in range(n)], [P.tok() for _ in range(n)]
        w1q, t_w1q = bufn([128, KC, 128], BF16)
        w3q, t_w3q = bufn([128, KC, 128], BF16)
        w2b, t_w2b = bufn([128, 4, D], BF16)
        hidT, t_hid = bufn([128, 4, 1024], BF16)
        wcb, t_wcb = bufn([128, 1024], F32)
        wce = AR.alloc([64, 1024], F32); t_wce = P.tok()
        sl, t_sl = bufn([128, 512], F32)
        t2, t_t2 = bufn([128, 512], F32)
        nq = 0
        ns = 0
        NE = GDBG.get("ne", NEXP)
        for th in range(2):
            tsl = slice(th * 1024, (th + 1) * 1024)
            P.dma("sp", u2h, u2T_v[:, :, tsl], reads=[t_u2T], writes=[t_u2h])
            for q4 in range(4):
                P.dma("sp", acc[:, q4 * 4:(q4 + 1) * 4, :], hT_v[:, q4 * 4:(q4 + 1) * 4, tsl],
                      reads=t_hT[4 * th:4 * th + 4], writes=t_acc[q4 * 4:(q4 + 1) * 4])
            for e in range(NE):
                i = e % 2
                TS("dve", wce, wcT[:, tsl], ident_f[0:64, e:e + 1], None, ALU.mult, None, [t_wcT, t_identf], [t_wce])
                for tg in range(2):
                    b = bank()
                    MM(PB[b][:, :], ones_f[0:64, :], wce[:, tg * 512:(tg + 1) * 512], True, True, [t_ones, t_wce], [PT[b]])
                    CP("act", wcb[i][:, tg * 512:(tg + 1) * 512], PB[b][:, :], [PT[b]], [t_wcb[i]])
                P.dma("pool", w2b[i], w2_ap[e].rearrange("(fc p) d -> p fc d", p=128), reads=[t_w2], writes=[t_w2b[i]])
                w1v = w1_ap[e].rearrange("(kc p) f -> p kc f", p=128)
                w3v = w3_ap[e].rearrange("(kc p) f -> p kc f", p=128)
                for fq in range(4):
                    jq = nq % 2
                    nq += 1
                    P.dma("pool", w1q[jq], w1v[:, :, fq * 128:(fq + 1) * 128], reads=[t_w1], writes=[t_w1q[jq]])
                    P.dma("pool", w3q[jq], w3v[:, :, fq * 128:(fq + 1) * 128], reads=[t_w3], writes=[t_w3q[jq]])
                    for tg in range(2):
                        js = ns % 2
                        ns += 1
                        b1 = bank()
                        for kc in range(KC):
                            MM(PB[b1][:, :], w1q[jq][:, kc, :], u2h[:, kc, tg * 512:(tg + 1) * 512], kc == 0, kc == KC - 1,
                               [t_w1q[jq], t_u2h], [PT[b1]])
                        b3 = bank()
                        for kc in range(KC):
                            MM(PB[b3][:, :], w3q[jq][:, kc, :], u2h[:, kc, tg * 512:(tg + 1) * 512], kc == 0, kc == KC - 1,
                               [t_w3q[jq], t_u2h], [PT[b3]])
                        ACT(sl[js], PB[b1][:, :], AF.Silu, [PT[b1]], [t_sl[js]])
                        TT("dve", t2[js], PB[b3][:, :], wcb[i][:, tg * 512:(tg + 1) * 512], ALU.mult, [PT[b3], t_wcb[i]], [t_t2[js]])
                        TT("dve", hidT[i][:, fq, tg * 512:(tg + 1) * 512], sl[js], t2[js], ALU.mult, [t_sl[js], t_t2[js]], [t_hid[i]])
                for dch in range(KC):
                    for tg in range(2):
                        by = bank()
                        for fc in range(4):
                            MM(PB[by][:, :], w2b[i][:, fc, dch * 128:(dch + 1) * 128], hidT[i][:, fc, tg * 512:(tg + 1) * 512],
                               fc == 0, fc == 3, [t_w2b[i], t_hid[i]], [PT[by]])
                        a_ = acc[:, dch, tg * 512:(tg + 1) * 512]
                        STT("dve", a_, PB[by][:, :], gc[:, dch:dch + 1], a_, ALU.mult, ALU.add, [PT[by], t_modc, t_acc[dch]], [t_acc[dch]])
            for q4 in range(4):
                P.dma("sp", hT_v[:, q4 * 4:(q4 + 1) * 4, tsl], acc[:, q4 * 4:(q4 + 1) * 4, :],
                      reads=t_acc[q4 * 4:(q4 + 1) * 4], writes=t_hT[4 * th:4 * th + 4])
        P.barrier()
        AR.release(m)

    u2T_d = dscr("u2T", [D, S], BF16)
    u2T_v = u2T_d.ap().rearrange("(kc p) t -> p kc t", p=128)
    t_u2T = P.tok("u2T")

    t_mix = P.tok("mixT")

    def outproj_stage(l, w_out_ap, t_wout):
        m = AR.mark()
        mx = [AR.alloc([128, KC, 512], BF16) for _ in range(2)]; t_mx = [P.tok() for _ in range(2)]
        wo = [AR.alloc([128, KC, 512], BF16) for _ in range(2)]; t_wo = [P.tok() for _ in range(2)]
        ht = [AR.alloc([128, 512], F32) for _ in range(3)]; t_ht = [P.tok() for _ in range(3)]
        gc = gate_col(l, 0)
        nw = 0
        nh = 0
        for tg in range(4):
            a, ta = mx[tg % 2], t_mx[tg % 2]
            P.dma("sp", a, mixT_v[:, :, tg * 512:(tg + 1) * 512], reads=[t_mix], writes=[ta])
            for db in range(4):
                wv, tw = wo[nw % 2], t_wo[nw % 2]
                nw += 1
                load_w(wv, w_out_ap, db * 512, 512, tw, t_wout)
                for j in range(4):
                    dch = db * 4 + j
                    h_, th_ = ht[nh % 3], t_ht[nh % 3]
                    nh += 1
                    P.dma("sp", h_, hT_v[:, dch, tg * 512:(tg + 1) * 512], reads=[t_hT[2 * tg], t_hT[2 * tg + 1]], writes=[th_])
                    b = bank()
                    for cc in range(KC):
                        MM(PB[b][:, :], wv[:, cc, j * 128:(j + 1) * 128], a[:, cc, :], cc == 0, cc == KC - 1, [tw, ta], [PT[b]])
                    STT("dve", h_, PB[b][:, :], gc[:, dch:dch + 1], h_, ALU.mult, ALU.add, [PT[b], t_modc, th_], [th_])
                    P.dma("sp", hT_v[:, dch, tg * 512:(tg + 1) * 512], h_, reads=[th_], writes=[t_hT[2 * tg], t_hT[2 * tg + 1]])
        P.barrier()
        AR.release(m)

    def final_stage():
        m = AR.mark()
        hg = [AR.alloc([128, KC, 128], F32) for _ in range(2)]; t_hg = [P.tok() for _ in range(2)]
        sq = [AR.alloc([128, 128], F32) for _ in range(3)]; t_sq = [P.tok() for _ in range(3)]
        rs = [AR.alloc([128, 128], F32) for _ in range(2)]; t_rs = [P.tok() for _ in range(2)]
        tmp = [AR.alloc([128, 128], F32) for _ in range(3)]; t_tmp = [P.tok() for _ in range(3)]
        ot = [AR.alloc([128, D], F32) for _ in range(2)]; t_ot = [P.tok() for _ in range(2)]
        n3 = 0
        for t in range(NT):
            h, th = hg[t % 2], t_hg[t % 2]
            o, to = ot[t % 2], t_ot[t % 2]
            P.dma("sp", h, hT_v[:, :, t * 128:(t + 1) * 128], reads=[t_hT[t // 2]], writes=[th])
            b = bank()
            for kc in range(KC):
                s_, ts_ = sq[n3 % 3], t_sq[n3 % 3]
                n3 += 1
                ACT(s_, h[:, kc, :], AF.Square, [th], [ts_])
                MM(PB[b][:, 0:128], ones_f, s_, kc == 0, kc == KC - 1, [t_ones, ts_], [PT[b]])
            r_, tr_ = rs[t % 2], t_rs[t % 2]
            TS("dve", r_, PB[b][:, 0:128], 1.0 / D, EPS, ALU.mult, ALU.add, [PT[b]], [tr_])
            RSQRT(r_, tr_)
            for kq in range(4):
                b2 = bank()
                for j in range(4):
                    kc = kq * 4 + j
                    t_, tt_ = tmp[n3 % 3], t_tmp[n3 % 3]
                    n3 += 1
                    STT("dve", t_, h[:, kc, :], finalg[:, kc:kc + 1], r_, ALU.mult, ALU.mult, [th, t_finalg, tr_], [tt_])
                    TR(PB[b2][:, j * 128:(j + 1) * 128], t_, ident_f, [tt_, t_identf], [PT[b2]])
                CP("act", o[:, kq * 512:(kq + 1) * 512], PB[b2][:, :], [PT[b2]], [to])
            P.dma("sp", out_d.ap()[t * 128:(t + 1) * 128, :], o, reads=[to], writes=[t_out])
        P.barrier()
        AR.release(m)

    t_out = P.tok("out")

    NL = L if "l1" in stages else 1
    gw = []
    for l in range(NL):
        g_ = {}
        g_["w_in"] = gather("w_in%d" % l, w_in_d.ap()[l], [D, INW])
        g_["w_out"] = gather("w_out%d" % l, w_out_d.ap()[l], [D, D])
        if "moe" in stages:
            g_["w1"] = gather("moe_w1_%d" % l, moe_w1_d.ap()[l], [NEXP, D, DEXP])
            g_["w3"] = gather("moe_w3_%d" % l, moe_w3_d.ap()[l], [NEXP, D, DEXP])
            g_["w2"] = gather("moe_w2_%d" % l, moe_w2_d.ap()[l], [NEXP, DEXP, D])
        gw.append(g_)
    if NL > 1 and R > 1 and "moe" in stages:
        P.cc_limit = 6
    for l in range(NL):
        w_in_ap, t_win = gw[l]["w_in"]
        w_out_ap, t_wout = gw[l]["w_out"]
        m_l = AR.mark()
        uT = AR.alloc([128, KC, S], BF16)
        norm_stage(l, 0)
        if "dbg_u" in dbg and l == 0:
            dbg_out["dbg_u"] = nc.dram_tensor("dbg_u", [128, KC, S], BF16, kind="ExternalOutput")
            P.dma("sp", dbg_out["dbg_u"].ap(), uT, reads=t_uT)
        if "gla" in stages:
            gla_stage(l, w_in_ap, t_win)
        if "lru" in stages:
            lru_stage(l, w_in_ap, t_win)
        if "diff" in stages:
            diff_stage(l, w_in_ap, t_win)
        P.barrier()
        AR.release(m_l)
        if "outproj" in stages:
            outproj_stage(l, w_out_ap, t_wout)
        if "moe" in stages:
            m_w = AR.mark()
            wcT = AR.alloc([64, S], F32); t_wcT = P.tok()
            m_u = AR.mark()
            uT = AR.alloc([128, KC, S], BF16)
            router = make_router(l, wcT, t_wcT)
            norm_stage(l, 1, router)
            P.dma("sp", u2T_v, uT, reads=t_uT, writes=[t_u2T])
            if "dbg_wc" in dbg and l == 0:
                dbg_out["dbg_wc"] = nc.dram_tensor("dbg_wc", [64, S], F32, kind="ExternalOutput")
                P.dma("sp", dbg_out["dbg_wc"].ap(), wcT, reads=[t_wcT])
            P.barrier()
            AR.release(m_u)
            moe_stage(l, gw[l]["w1"][0], gw[l]["w3"][0], gw[l]["w2"][0], gw[l]["w1"][1], gw[l]["w3"][1], gw[l]["w2"][1], wcT, t_wcT)
            AR.release(m_w)
            P.cc_limit = 10 ** 9
    if "dbg_mix" in dbg:
        dbg_out["dbg_mix"] = nc.dram_tensor("dbg_mix", [D, S], BF16, kind="ExternalOutput")
        P.dma("sp", dbg_out["dbg_mix"].ap(), mixT_d.ap(), reads=[t_mix])
    if "dbg_h" in dbg:
        dbg_out["dbg_h"] = nc.dram_tensor("dbg_h", [D, S], F32, kind="ExternalOutput")
        P.dma("sp", dbg_out["dbg_h"].ap(), hT_d.ap(), reads=t_hT)
    if "final" in stages:
        final_stage()
    P.emit()
    st.close()
    print("arena peak", AR.peak, "instr", {k: v.n for k, v in P.eng.items()})
    return nc


def _col(v):
    v = np.asarray(v, np.float32)
    lead = v.shape[:-1]
    n = v.shape[-1] // 128
    v = v.reshape(*lead, n, 128)
    return np.ascontiguousarray(np.moveaxis(v, -1, 0))


def _t5_bucket_np(rel):
    nb = 16
    ret = (rel > 0).astype(np.int32) * nb
    n = np.abs(rel)
    max_exact = nb // 2
    nf = np.maximum(n, 1).astype(np.float32)
    large = max_exact + (np.log(nf / max_exact) / math.log(128 / max_exact) * (nb - max_exact)).astype(np.int32)
    large = np.minimum(large, nb - 1)
    return ret + np.where(n < max_exact, n, large)


def const_inputs(stages):
    c = {}
    c["ident_f"] = np.eye(128, dtype=np.float32)
    c["ones_f"] = np.ones((128, 128), np.float32)
    if "gla" in stages:
        tp = np.arange(64)
        triC = (tp[:, None] <= tp[None, :]).astype(np.float32) / 16.0
        triR = (tp[:, None] > tp[None, :]).astype(np.float32) / 16.0
        full = np.ones((64, 64), np.float32) / 16.0
        c["gla_tri"] = np.stack([triC, triR, full]).astype(np.float32)
        mf = (tp[:, None] <= tp[None, :]).astype(np.float32)
        mb = (tp[:, None] > tp[None, :]).astype(np.float32)
        c["gla_mask"] = np.stack([np.tile(mf, (1, 3)), np.tile(mb, (1, 3))]).astype(np.float32)
    if "diff" in stages:
        k = np.arange(128)[:, None]
        jj = np.arange(256)[None, :]
        bkt = _t5_bucket_np((k - jj).astype(np.int32))
        oh = (bkt[:, None, :] == np.arange(32)[None, :, None]).astype(np.float32)
        c["t5_oh"] = np.ascontiguousarray(oh)
        mask = np.zeros((128, 256), np.float32)
        mask[64:, :64] = -200.0
        c["t5_mask"] = mask
    return c


def core_inputs(inp, b, R, stages):
    r = b % R
    JC = 96 // R
    NCOL = 12288 // R
    m = {}
    m["x"] = np.ascontiguousarray(inp["x"][b])
    m["c_col"] = np.ascontiguousarray(_col(inp["c"]).transpose(0, 2, 1))
    oh = np.zeros((128, R * L * JC, 8), np.float32)
    oh[:, :, b] = 1.0
    m["onehot_rep"] = oh
    m["ada_w_sh"] = np.ascontiguousarray(inp["ada_w"][:, :, r * NCOL:(r + 1) * NCOL])
    m["ada_b_col"] = _col(inp["ada_b"])
    m["norm_g_col"] = np.ascontiguousarray(np.stack([_col(inp["norm1_g"]), _col(inp["norm2_g"])], axis=2))
    m["final_g_col"] = _col(inp["final_g"])
    rows = D // R
    m["w_in_sh"] = np.ascontiguousarray(inp["w_in"][:, r * rows:(r + 1) * rows, :])
    m["w_out_sh"] = np.ascontiguousarray(inp["w_out"][:, r * rows:(r + 1) * rows, :])
    if "lru" in stages:
        cols = [inp["lru_conv_w"][:, i, :] for i in range(4)] + [inp["lru_conv_b"], inp["lru_ba"], inp["lru_bx"], inp["lru_lambda"]]
        m["lru_col"] = np.ascontiguousarray(np.stack([_col(v) for v in cols], axis=-1))
        wbd = np.zeros((L, 4, 2, 128, 128), np.float32)
        for wi, nm in enumerate(("lru_wa", "lru_wx")):
            for cc in range(4):
                for hb in range(2):
                    wbd[:, cc, wi, hb * 64:(hb + 1) * 64, hb * 64:(hb + 1) * 64] = inp[nm][:, 2 * cc + hb]
        m["lru_wbd"] = wbd
    if "gla" in stages:
        m["gla_wa2"] = np.ascontiguousarray(np.concatenate([inp["gla_w_a2"], inp["gla_b_a"][:, None, :]], axis=1))
        m["gla_ng"] = np.ascontiguousarray(inp["gla_norm_g"])
    if "diff" in stages:
        m["diff_l"] = np.ascontiguousarray(np.concatenate([inp["diff_lq1"], inp["diff_lk1"], inp["diff_lq2"], inp["diff_lk2"]], axis=1))
        m["diff_subln"] = np.ascontiguousarray(inp["diff_subln_g"])
        m["rel_bias"] = np.ascontiguousarray(inp["rel_bias"].reshape(1, 192))
    if "moe" in stages:
        m["router_w"] = np.ascontiguousarray(np.concatenate([inp["router_g_w"], inp["router_e_w"]], axis=2))
        m["router_b"] = np.ascontiguousarray(np.concatenate([inp["router_g_b"], inp["router_e_b"]], axis=1))
        ne = NEXP // R
        for nm in ("moe_w1", "moe_w3", "moe_w2"):
            m[nm + "_sh"] = np.ascontiguousarray(inp[nm][:, r * ne:(r + 1) * ne])
    m.update(const_inputs(stages))
    return m


ALL_STAGES = {"gla", "lru", "diff", "outproj", "moe", "final", "l1"}


def kernel(**inputs):
    R = 8
    inp = {k: np.asarray(v) for k, v in inputs.items()}
    nc = build(R, ALL_STAGES)
    in_maps = [core_inputs(inp, b, R, ALL_STAGES) for b in range(R)]
    res = run_bass_kernel_spmd(nc, in_maps, core_ids=list(range(R)))
    out = np.stack([np.asarray(res.results[b]["out"]) for b in range(R)], axis=0)
    return np.ascontiguousarray(out.astype(np.float32))
```
